# Optimizing a Trainium2 kernel written in Bass

```python
import math
import jax, jax.numpy as jnp
from jax import lax
import numpy as np

D_MODEL = 2048
BATCH = 16
SEQ = 2048
DEPTH = 4

N_META = 16
GRID_W = 64
Q_BLOCK = 128
ROPE_THETA = 10000.0
EPS = 1e-6

MLA_HEADS = 8
Q_LORA = 512
KV_LORA = 256
NOPE_DIM = 128
ROPE_DIM = 64
V_DIM = 128
QK_HEAD = NOPE_DIM + ROPE_DIM
MLA_WIDTH = MLA_HEADS * V_DIM
MLA_SCALE = 1.0 / math.sqrt(QK_HEAD)

GQA_HEADS = 8
GQA_KV_HEADS = 2
GQA_HEAD_DIM = 128
GQA_WIDTH = GQA_HEADS * GQA_HEAD_DIM
GQA_SCALE = 1.0 / math.sqrt(GQA_HEAD_DIM)

MIX_WIDTH = MLA_WIDTH + GQA_WIDTH

OFF_CQ = 0
OFF_CKV = OFF_CQ + Q_LORA
OFF_KR = OFF_CKV + KV_LORA
OFF_GQ = OFF_KR + ROPE_DIM
OFF_GK = OFF_GQ + GQA_HEADS * GQA_HEAD_DIM
OFF_GV = OFF_GK + GQA_KV_HEADS * GQA_HEAD_DIM
IN_COLS = OFF_GV + GQA_KV_HEADS * GQA_HEAD_DIM

N_EXPERTS = 16
N_GROUPS = 4
EXPERTS_PER_GROUP = N_EXPERTS // N_GROUPS
TOP_K = 2
EXPERT_FF = 1024

ALPHA = (2.0 * DEPTH) ** 0.25
BETA = (8.0 * DEPTH) ** -0.25

kernel_name = "hybrid_mla_gqa_axial_grouped_moe_deepnorm_encoder"


def layer_norm(x, g, b):
    xf = x.astype(jnp.float32)
    mu = jnp.mean(xf, axis=-1, keepdims=True)
    xc = xf - mu
    var = jnp.mean(xc * xc, axis=-1, keepdims=True)
    y = xc * lax.rsqrt(var + EPS) * g.astype(jnp.float32) + b.astype(jnp.float32)
    return y.astype(x.dtype)


def rms_norm(x, g):
    xf = x.astype(jnp.float32)
    y = xf * lax.rsqrt(jnp.mean(xf * xf, axis=-1, keepdims=True) + EPS) * g.astype(jnp.float32)
    return y.astype(x.dtype)


def axial_rope_tables(pos_row, pos_col, rot_dim):
    n = rot_dim // 4
    inv = ROPE_THETA ** (-jnp.arange(n, dtype=jnp.float32) / n)
    ang = jnp.concatenate([pos_row[:, None] * inv, pos_col[:, None] * inv], axis=-1)
    return jnp.cos(ang), jnp.sin(ang)


def apply_rope(x, cos, sin):
    half = x.shape[-1] // 2
    x1, x2 = x[..., :half], x[..., half:]
    c = cos[None, :, None, :].astype(x.dtype)
    s = sin[None, :, None, :].astype(x.dtype)
    return jnp.concatenate([x1 * c - x2 * s, x1 * s + x2 * c], axis=-1)


def bidir_attention(q, k, v, scale):
    B, T, Hq, dk = q.shape
    Hk, dv = k.shape[2], v.shape[-1]
    G = Hq // Hk
    S = T - N_META
    qg = q.reshape(B, T, Hk, G, dk)

    def attend(qb):
        s = jnp.einsum('bqhgd,bkhd->bhgqk', qb, k).astype(jnp.float32) * scale
        p = jax.nn.softmax(s, axis=-1).astype(v.dtype)
        return jnp.einsum('bhgqk,bkhd->bqhgd', p, v)

    out_meta = attend(qg[:, :N_META])
    nb = S // Q_BLOCK
    q_real = qg[:, N_META:].reshape(B, nb, Q_BLOCK, Hk, G, dk).transpose(1, 0, 2, 3, 4, 5)
    out_real = lax.map(attend, q_real)
    out_real = out_real.transpose(1, 0, 2, 3, 4, 5).reshape(B, S, Hk, G, dv)
    out = jnp.concatenate([out_meta, out_real], axis=1)
    return out.reshape(B, T, Hq, dv)


def grouped_moe(h, w_router, router_bias, w_gate, w_up, w_down):
    B, T, D = h.shape
    t = h.reshape(B * T, D)
    scores = jax.nn.sigmoid((t @ w_router).astype(jnp.float32))
    sel = scores + router_bias.astype(jnp.float32)
    grp_score = lax.top_k(sel.reshape(-1, N_GROUPS, EXPERTS_PER_GROUP), 2)[0].sum(-1)
    g_idx = jnp.argmax(grp_score, axis=-1)
    in_group = (jnp.arange(N_EXPERTS) // EXPERTS_PER_GROUP)[None, :] == g_idx[:, None]
    masked = jnp.where(in_group, sel, -jnp.inf)
    _, e_idx = lax.top_k(masked, TOP_K)
    w = jnp.take_along_axis(scores, e_idx, axis=-1)
    w = w / jnp.sum(w, axis=-1, keepdims=True)
    combine = jnp.sum(jax.nn.one_hot(e_idx, N_EXPERTS, dtype=jnp.float32) * w[..., None], axis=1)
    combine = combine.astype(t.dtype)
    y = jnp.zeros_like(t)
    for e in range(N_EXPERTS):
        he = jax.nn.silu(t @ w_gate[e]) * (t @ w_up[e])
        y = y + combine[:, e:e + 1] * (he @ w_down[e])
    return y.reshape(B, T, D)


def setup_inputs(seed: int = 0) -> dict:
    key = jax.random.key(seed)
    ks = jax.random.split(key, 23)

    def nrm(k, shape, scale):
        return jax.random.normal(k, shape, jnp.float32) * scale

    def gain(k, shape):
        return 1.0 + 0.05 * jax.random.normal(k, shape, jnp.float32)

    return {
        "x": nrm(ks[0], (BATCH, SEQ, D_MODEL), 1.0),
        "meta_tokens": nrm(ks[1], (N_META, D_MODEL), 1.0),
        "ln_in_g": gain(ks[2], (D_MODEL,)),
        "ln_in_b": nrm(ks[3], (D_MODEL,), 0.02),
        "w_in": nrm(ks[4], (DEPTH, D_MODEL, IN_COLS), D_MODEL ** -0.5),
        "g_q_lora": gain(ks[5], (DEPTH, Q_LORA)),
        "w_q_b": nrm(ks[6], (DEPTH, Q_LORA, MLA_HEADS * QK_HEAD), Q_LORA ** -0.5),
        "g_kv_lora": gain(ks[7], (DEPTH, KV_LORA)),
        "w_kv_b": nrm(ks[8], (DEPTH, KV_LORA, MLA_HEADS * (NOPE_DIM + V_DIM)), KV_LORA ** -0.5),
        "g_qk_q": gain(ks[9], (DEPTH, GQA_HEAD_DIM)),
        "g_qk_k": gain(ks[10], (DEPTH, GQA_HEAD_DIM)),
        "g_out_mla": gain(ks[11], (DEPTH, MLA_WIDTH)),
        "g_out_gqa": gain(ks[12], (DEPTH, GQA_WIDTH)),
        "w_out": nrm(ks[13], (DEPTH, MIX_WIDTH, D_MODEL), MIX_WIDTH ** -0.5 * BETA),
        "ln1_g": gain(ks[14], (DEPTH, D_MODEL)),
        "ln1_b": nrm(ks[15], (DEPTH, D_MODEL), 0.02),
        "w_router": nrm(ks[16], (D_MODEL, N_EXPERTS), D_MODEL ** -0.5),
        "router_bias": nrm(ks[17], (N_EXPERTS,), 0.01),
        "w_gate": nrm(ks[18], (DEPTH, N_EXPERTS, D_MODEL, EXPERT_FF), D_MODEL ** -0.5),
        "w_up": nrm(ks[19], (DEPTH, N_EXPERTS, D_MODEL, EXPERT_FF), D_MODEL ** -0.5),
        "w_down": nrm(ks[20], (DEPTH, N_EXPERTS, EXPERT_FF, D_MODEL), EXPERT_FF ** -0.5 * BETA),
        "ln2_g": gain(ks[21], (DEPTH, D_MODEL)),
        "ln2_b": nrm(ks[22], (DEPTH, D_MODEL), 0.02),
    }


def reference(x, meta_tokens, ln_in_g, ln_in_b, w_in, g_q_lora, w_q_b, g_kv_lora, w_kv_b,
              g_qk_q, g_qk_k, g_out_mla, g_out_gqa, w_out, ln1_g, ln1_b,
              w_router, router_bias, w_gate, w_up, w_down, ln2_g, ln2_b):
    B, S, D = x.shape
    T = N_META + S
    ROWS = S // GRID_W

    pos_row = jnp.concatenate([jnp.full((N_META,), -1.0, jnp.float32),
                               jnp.repeat(jnp.arange(ROWS, dtype=jnp.float32), GRID_W)])
    pos_col = jnp.concatenate([jnp.arange(N_META, dtype=jnp.float32),
                               jnp.tile(jnp.arange(GRID_W, dtype=jnp.float32), ROWS)])
    cos_a, sin_a = axial_rope_tables(pos_row, pos_col, ROPE_DIM)
    cos_b, sin_b = axial_rope_tables(pos_row, pos_col, GQA_HEAD_DIM)

    meta = jnp.broadcast_to(meta_tokens[None].astype(x.dtype), (B, N_META, D))
    h = layer_norm(jnp.concatenate([meta, x], axis=1), ln_in_g, ln_in_b)

    for l in range(DEPTH):
        u = h @ w_in[l]

        c_q = rms_norm(u[..., OFF_CQ:OFF_CKV], g_q_lora[l])
        q_a = (c_q @ w_q_b[l]).reshape(B, T, MLA_HEADS, QK_HEAD)
        q_nope, q_rope = q_a[..., :NOPE_DIM], apply_rope(q_a[..., NOPE_DIM:], cos_a, sin_a)
        c_kv = rms_norm(u[..., OFF_CKV:OFF_KR], g_kv_lora[l])
        kv = (c_kv @ w_kv_b[l]).reshape(B, T, MLA_HEADS, NOPE_DIM + V_DIM)
        k_nope, v_a = kv[..., :NOPE_DIM], kv[..., NOPE_DIM:]
        k_rope = apply_rope(u[..., OFF_KR:OFF_GQ].reshape(B, T, 1, ROPE_DIM), cos_a, sin_a)
        q_full = jnp.concatenate([q_nope, q_rope], axis=-1)
        k_full = jnp.concatenate([k_nope, jnp.broadcast_to(k_rope, (B, T, MLA_HEADS, ROPE_DIM))], axis=-1)
        o_a = bidir_attention(q_full, k_full, v_a, MLA_SCALE).reshape(B, T, MLA_WIDTH)

        q_b = rms_norm(u[..., OFF_GQ:OFF_GK].reshape(B, T, GQA_HEADS, GQA_HEAD_DIM), g_qk_q[l])
        k_b = rms_norm(u[..., OFF_GK:OFF_GV].reshape(B, T, GQA_KV_HEADS, GQA_HEAD_DIM), g_qk_k[l])
        v_b = u[..., OFF_GV:IN_COLS].reshape(B, T, GQA_KV_HEADS, GQA_HEAD_DIM)
        q_b = apply_rope(q_b, cos_b, sin_b)
        k_b = apply_rope(k_b, cos_b, sin_b)
        o_b = bidir_attention(q_b, k_b, v_b, GQA_SCALE).reshape(B, T, GQA_WIDTH)

        mixed = jnp.concatenate([rms_norm(o_a, g_out_mla[l]), rms_norm(o_b, g_out_gqa[l])], axis=-1) @ w_out[l]
        h = layer_norm(ALPHA * h + mixed, ln1_g[l], ln1_b[l])

        ffn = grouped_moe(h, w_router, router_bias, w_gate[l], w_up[l], w_down[l])
        h = layer_norm(ALPHA * h + ffn, ln2_g[l], ln2_b[l])

    return h[:, N_META:]
```

```python
import math
from contextlib import ExitStack
import numpy as np
import concourse.bass as bass
import concourse.mybir as mybir
from concourse.bass_utils import run_bass_kernel_spmd

F32 = mybir.dt.float32
BF16 = mybir.dt.bfloat16
AF = mybir.ActivationFunctionType
ALU = mybir.AluOpType
AX = mybir.AxisListType

D = 2048
SEQ = 2048
NMETA = 16
T = SEQ + NMETA
DEPTH = 4
INC = 2368
EPS = 1e-6
ALPHA = (2.0 * DEPTH) ** 0.25
MLA_SCALE = 1.0 / math.sqrt(192.0)
GQA_SCALE = 1.0 / math.sqrt(128.0)
NE = 16
FF = 1024
NCOL = 88
C_GQ, C_GKV, C_GQQ, C_GQK, C_GOA, C_GOB, C_L1G, C_L1B, C_L2G, C_L2B = 0, 4, 6, 7, 8, 16, 24, 40, 56, 72
NEG = -1.0e30

SPC_DEFAULT = 4


class St:
    def __init__(self, eng, sem, own=True):
        self.eng, self.sem, self.n, self.seen, self.own = eng, sem, 0, {}, own


class DSem:
    def __init__(self, sem):
        self.sem, self.cnt = sem, 0


class TB:
    def __init__(self, name, d=None):
        self.name, self.w, self.r, self.d = name, None, {}, d


class K:
    def __init__(self, nc, es):
        self.nc = nc
        sems = [es.enter_context(nc.semaphore(f"s{i}")) for i in range(40)]
        self.PE = St(nc.tensor, sems[0], own=False)
        self.ACT = St(nc.scalar, sems[1])
        self.DVE = St(nc.vector, sems[2])
        self.POOL = St(nc.gpsimd, sems[3])
        self.SP = St(nc.sync, sems[4])
        self.streams = [self.PE, self.ACT, self.DVE, self.POOL, self.SP]
        self.dpool = [DSem(s) for s in sems[5:30]]
        self.ppool = [DSem(s) for s in sems[30:]]
        self.dnext = 0
        self.pnext = 0
        self.pes = None

    def _wait(self, st, ev):
        if ev is None:
            return
        if ev[0] == "d":
            sem, val = ev[1].sem, 16 * ev[1].cnt
        else:
            sem, val = ev[1], ev[2]
            if sem is st.sem and not st.own:
                return
        if st.seen.get(sem.num, 0) >= val:
            return
        st.eng.wait_ge(sem, val)
        st.seen[sem.num] = val

    def dep(self, st, reads, writes):
        for b in reads:
            self._wait(st, b.w)
        for b in writes:
            self._wait(st, b.w)
            for ev in list(b.r.values()):
                self._wait(st, ev)

    def fin(self, ev, key, reads, writes):
        for b in reads:
            b.r[key] = ev
        for b in writes:
            b.w = ev
            b.r = {}

    def op(self, st, reads, writes, fn):
        self.dep(st, reads, writes)
        ins = fn(st.eng)
        st.n += 1
        ins.then_inc(st.sem, 1)
        self.fin(("c", st.sem, st.n), st.sem.num, reads, writes)

    def dma(self, q, out, in_, reads, writes, d):
        self.dep(q, reads, writes)
        ins = q.eng.dma_start(out=out, in_=in_)
        d.cnt += 1
        ins.then_inc(d.sem, 16)
        self.fin(("d", d), d.sem.num, reads, writes)

    def begin(self):
        self.pes = ExitStack()
        self.dnext = 0
        self.pnext = 0

    def sb(self, name, shape, dt, dma=False):
        self.uid = getattr(self, "uid", 0) + 1
        t = self.pes.enter_context(self.nc.sbuf_tensor(f"{name}_u{self.uid}", shape, dt))
        d = None
        if dma == "pool":
            d = self.ppool[self.pnext]
            self.pnext += 1
        elif dma:
            d = self.dpool[self.dnext]
            self.dnext += 1
        return t, TB(name, d)

    def barrier(self):
        evs = [("c", s.sem, s.n) for s in self.streams if s.n > 0]
        evs += [("d", d) for d in self.dpool + self.ppool if d.cnt > 0]
        for s in self.streams:
            for ev in evs:
                self._wait(s, ev)

    def end(self):
        self.barrier()
        self.pes.close()
        self.pes = None


def build_nc(SPC=SPC_DEFAULT, depth=DEPTH, dbg=False):
    nc = bass.Bass("TRN2", target_bir_lowering=False)
    NR = SPC * SEQ
    NM = SPC * NMETA
    NT = NR + NM

    def din(name, shape, dt=F32):
        return nc.dram_tensor(name, list(shape), dt, kind="ExternalInput").ap()

    x = din("x", [SPC, SEQ, D])
    meta = din("meta", [NMETA, D])
    lng = din("lng", [D])
    lnb = din("lnb", [D])
    w_in = din("w_in", [depth, D, INC])
    w_qb = din("w_qb", [depth, 512, 1536])
    w_kvb = din("w_kvb", [depth, 256, 2048])
    w_out = din("w_out", [depth, D, D])
    w_rt = din("w_rt", [D, NE])
    rbias = din("rbias", [NE])
    w_gate = din("w_gate", [depth, NE, D, FF])
    w_up = din("w_up", [depth, NE, D, FF])
    w_down = din("w_down", [depth, NE, FF, D])
    cols_d = din("cols", [128, depth * NCOL])
    ident_d = din("ident", [128, 128])
    rmat_d = din("rmat", [2, 128, 128])
    sel_d = din("sel", [NE, NE * 128])
    tabA = din("tabA", [2, 128, NT])
    tabB = din("tabB", [2, 128, NT])
    out = nc.dram_tensor("out", [SPC, SEQ, D], F32, kind="ExternalOutput").ap()

    okind = "ExternalOutput" if dbg else "Internal"

    def dscr(name, shape, dt):
        return nc.dram_tensor(name, list(shape), dt, kind=okind).ap()

    HT32 = dscr("HT32", [D, NT], F32)
    HTb = dscr("HTb", [D, NT], BF16)
    QaT = dscr("QaT", [8, 128, NT], BF16)
    QrT = dscr("QrT", [4, 128, NT], BF16)
    KaT = dscr("KaT", [8, 128, NT], BF16)
    KrT = dscr("KrT", [64, NT], BF16)
    Va = dscr("Va", [NT, 1024], BF16)
    GQT = dscr("GQT", [8, 128, NT], BF16)
    GKT = dscr("GKT", [2, 128, NT], BF16)
    GV = dscr("GV", [NT, 256], BF16)
    OT = dscr("OT", [D, NT], F32)

    def fm(ap2d):
        return ap2d.rearrange("(kc p) t -> p kc t", p=128)

    with ExitStack() as es:
        k = K(nc, es)
        PE, ACT, DVE, POOL, SP = k.PE, k.ACT, k.DVE, k.POOL, k.SP
        ps = [es.enter_context(nc.psum_tensor(f"ps{i}", [128, 512], F32)) for i in range(8)]
        PT = [TB(f"ps{i}") for i in range(8)]

        def gsb(name, shape, dt):
            return es.enter_context(nc.sbuf_tensor("g_" + name, shape, dt))

        gd = k.dpool.pop()
        ident = gsb("ident", [128, 128], F32)
        ones_f = gsb("ones_f", [128, 128], F32)
        ones_b = gsb("ones_b", [128, 128], BF16)
        rmat = gsb("rmat", [128, 2, 128], BF16)
        epsc = gsb("epsc", [128, 1], F32)
        eps2c = gsb("eps2c", [128, 1], F32)
        sel = gsb("sel", [NE, NE * 128], F32)
        cols = gsb("cols", [128, depth * NCOL], F32)
        rb_bc = gsb("rb_bc", [128, NE], F32)
        wrt = gsb("wrt", [128, 16, NE], F32)
        CONST = TB("const", gd)
        k.dma(SP, ident[:], ident_d[:, :], [], [CONST], gd)
        k.dma(SP, sel[:], sel_d[:, :], [], [CONST], gd)
        k.dma(SP, cols[:], cols_d[:, :], [], [CONST], gd)
        k.dma(SP, rb_bc[:], rbias.partition_broadcast(128), [], [CONST], gd)
        k.dma(SP, wrt[:], w_rt.rearrange("(kc p) e -> p kc e", p=128), [], [CONST], gd)
        gdp = k.ppool.pop()
        CONSTP = TB("constp", gdp)
        k.dma(POOL, rmat[:], rmat_d.rearrange("c p m -> p c m"), [], [CONSTP], gdp)
        CM = TB("constmem")
        k.op(DVE, [], [CM], lambda e: e.memset(ones_f[:], 1.0))
        k.op(DVE, [], [CM], lambda e: e.memset(ones_b[:], 1.0))
        k.op(DVE, [], [CM], lambda e: e.memset(epsc[:], EPS))
        k.op(DVE, [], [CM], lambda e: e.memset(eps2c[:], EPS / (ALPHA * ALPHA)))
        CR = [CONST, CONSTP, CM]

        def mm(pt, out_ap, pairs, reads, start=True, stop=True):
            def f(e):
                ins = None
                n = len(pairs)
                for i, (l, r) in enumerate(pairs):
                    ins = e.matmul(out_ap, l, r, start=(start and i == 0), stop=(stop and i == n - 1))
                return ins
            k.op(PE, reads + CR, [pt], f)

        def feature_blocks():
            bl = [(i * 512, 512) for i in range(NR // 512)]
            bl.append((NR, NM))
            return bl

        def rsqrt_from(pt, ps_ap, out_tb, out_ap, scale, eps_ap):
            k.op(ACT, [pt] + CR, [out_tb], lambda e: e.activation(out=out_ap, in_=ps_ap, func=AF.Sqrt, bias=eps_ap, scale=scale))
            k.op(DVE, [out_tb], [out_tb], lambda e: e.reciprocal(out=out_ap, in_=out_ap))

        def phase0():
            k.begin()
            xt = [k.sb(f"xt{i}", [128, D], F32, dma=True) for i in range(2)]
            hb, HB = k.sb("hb", [128, D], F32)
            gbc, GB = k.sb("gbc", [128, D], F32, dma=True)
            bbc, BB = k.sb("bbc", [128, D], F32, dma=True)
            stf, STF = k.sb("stf", [128, 16, 512], F32, dma=True)
            stb, STB = k.sb("stb", [128, 16, 512], BF16, dma=True)
            sm, SM = k.sb("sm", [128, 8], F32)
            k.dma(SP, gbc[:], lng.partition_broadcast(128), [], [GB], GB.d)
            k.dma(SP, bbc[:], lnb.partition_broadcast(128), [], [BB], BB.d)
            it = 0
            import os as _os
            for (t0, n) in feature_blocks():
                if _os.environ.get("P0_SKIP_META") and n < 128:
                    continue
                ntile = (n + 127) // 128
                for j in range(ntile):
                    p = min(128, n - j * 128)
                    xs, XS = xt[it % 2]
                    it += 1
                    if t0 < NR:
                        s, s0 = divmod(t0 + j * 128, SEQ)
                        k.dma(SP, xs[0:p, :], x[s, s0:s0 + p, :], [], [XS], XS.d)
                    else:
                        for s in range(SPC):
                            k.dma(SP, xs[16 * s:16 * s + 16, :], meta[:, :], [], [XS], XS.d)
                    _step = int(_os.environ.get("P0_STEP", "99"))
                    if _step <= 1:
                        k.end(); return
                    k.op(DVE, [], [SM], lambda e: e.memset(sm[0:p, 0:2], 0.0))
                    k.op(ACT, [XS], [HB, SM], lambda e: e.activation(out=hb[0:p, :], in_=xs[0:p, :], func=AF.Identity, accum_out=sm[0:p, 0:1]))
                    k.op(ACT, [XS], [HB, SM], lambda e: e.activation(out=hb[0:p, :], in_=xs[0:p, :], func=AF.Square, accum_out=sm[0:p, 1:2]))
                    if _step <= 2:
                        k.end(); return
                    k.op(DVE, [SM], [SM], lambda e: e.tensor_scalar(out=sm[0:p, 2:3], in0=sm[0:p, 0:1], scalar1=1.0 / D, scalar2=None, op0=ALU.mult))
                    k.op(DVE, [SM], [SM], lambda e: e.tensor_tensor(out=sm[0:p, 3:4], in0=sm[0:p, 2:3], in1=sm[0:p, 2:3], op=ALU.mult))
                    k.op(DVE, [SM], [SM], lambda e: e.scalar_tensor_tensor(out=sm[0:p, 4:5], in0=sm[0:p, 1:2], scalar=1.0 / D, in1=sm[0:p, 3:4], op0=ALU.mult, op1=ALU.subtract))
                    k.op(ACT, [SM] + CR, [SM], lambda e: e.activation(out=sm[0:p, 5:6], in_=sm[0:p, 4:5], func=AF.Sqrt, bias=epsc[0:p, :], scale=1.0))
                    k.op(DVE, [SM], [SM], lambda e: e.reciprocal(out=sm[0:p, 5:6], in_=sm[0:p, 5:6]))
                    k.op(DVE, [SM], [SM], lambda e: e.scalar_tensor_tensor(out=sm[0:p, 6:7], in0=sm[0:p, 2:3], scalar=-1.0, in1=sm[0:p, 5:6], op0=ALU.mult, op1=ALU.mult))
                    if _step <= 3:
                        k.end(); return
                    k.op(ACT, [XS, SM], [HB], lambda e: e.activation(out=hb[0:p, :], in_=xs[0:p, :], func=AF.Identity, bias=sm[0:p, 6:7], scale=sm[0:p, 5:6]))
                    k.op(DVE, [HB, GB], [HB], lambda e: e.tensor_tensor(out=hb[0:p, :], in0=hb[0:p, :], in1=gbc[0:p, :], op=ALU.mult))
                    k.op(DVE, [HB, BB], [HB], lambda e: e.tensor_tensor(out=hb[0:p, :], in0=hb[0:p, :], in1=bbc[0:p, :], op=ALU.add))
                    if _step <= 4:
                        k.end(); return
                    for b4 in range(4):
                        def tr(e, b4=b4):
                            ins = None
                            for q in range(4):
                                kc = b4 * 4 + q
                                ins = e.transpose(ps[b4][:, q * 128:q * 128 + p], hb[0:p, kc * 128:(kc + 1) * 128], ident[0:p, 0:p])
                            return ins
                        k.op(PE, [HB] + CR, [PT[b4]], tr)
                        for q in range(4):
                            kc = b4 * 4 + q
                            src = ps[b4][:, q * 128:q * 128 + p]
                            k.op(ACT, [PT[b4]], [STF], lambda e, src=src, kc=kc: e.activation(out=stf[:, kc, j * 128:j * 128 + p], in_=src, func=AF.Identity))
                            k.op(DVE, [STF], [STB], lambda e, kc=kc: e.tensor_copy(out=stb[:, kc, j * 128:j * 128 + p], in_=stf[:, kc, j * 128:j * 128 + p]))
                    if _step <= 5:
                        k.end(); return
                if _step <= 6:
                    k.end(); return
                k.dma(SP, fm(HT32[:, t0:t0 + n]), stf[:, :, 0:n], [STF], [], STF.d)
                k.dma(SP, fm(HTb[:, t0:t0 + n]), stb[:, :, 0:n], [STB], [], STB.d)
            k.end()

        def rope_unit(P, n, xg, XG, ri, tab, TAB, t1, T1, t2, T2, out_tb, out_ap, pr, rr=None, RR=None):
            mm(PT[pr], ps[pr][0:P, 0:n], [(rmat[0:P, ri, 0:P], xg[0:P, 0:n])], [XG])
            k.op(POOL, [XG, TAB], [T1], lambda e: e.tensor_tensor(out=t1[0:P, 0:n], in0=xg[0:P, 0:n], in1=tab[0:P, 0, 0:n], op=ALU.mult))
            k.op(DVE, [PT[pr], TAB], [T2], lambda e: e.tensor_tensor(out=t2[0:P, 0:n], in0=ps[pr][0:P, 0:n], in1=tab[0:P, 1, 0:n], op=ALU.mult))
            if rr is None:
                k.op(POOL, [T1, T2], [out_tb], lambda e: e.tensor_tensor(out=out_ap, in0=t1[0:P, 0:n], in1=t2[0:P, 0:n], op=ALU.add))
            else:
                k.op(POOL, [T1, T2], [T1], lambda e: e.tensor_tensor(out=t1[0:P, 0:n], in0=t1[0:P, 0:n], in1=t2[0:P, 0:n], op=ALU.add))
                k.op(DVE, [T1, RR], [out_tb], lambda e: e.tensor_tensor(out=out_ap, in0=t1[0:P, 0:n], in1=rr[0:P, 0:n], op=ALU.mult))

        def phaseA1(l):
            k.begin()
            co = l * NCOL
            wA, WA = k.sb("wA", [128, 16, 832], BF16, dma="pool")
            wq, _ = k.sb("wq", [128, 4, 1536], BF16)
            wkv, _ = k.sb("wkv", [128, 2, 2048], BF16)
            k.dma(POOL, wA[:], fm(w_in[l, :, 0:832]), [], [WA], WA.d)
            k.dma(POOL, wq[:], fm(w_qb[l]), [], [WA], WA.d)
            k.dma(POOL, wkv[:], fm(w_kvb[l]), [], [WA], WA.d)
            hts = [k.sb(f"hT{i}", [128, 16, 512], BF16, dma=True) for i in range(2)]
            tas = [k.sb(f"ta{i}", [128, 2, 512], F32, dma=True) for i in range(2)]
            cqg, CQG = k.sb("cqg", [128, 4, 512], BF16)
            ckg, CKG = k.sb("ckg", [128, 2, 512], BF16)
            sqk, SQK = k.sb("sqk", [128, 2, 512], BF16)
            sqb = [k.sb(f"sqb{i}", [128, 512], BF16) for i in range(2)]
            xr = [k.sb(f"xr{i}", [128, 512], BF16) for i in range(2)]
            rq, RQ = k.sb("rq", [128, 512], F32)
            rkv, RKV = k.sb("rkv", [128, 512], F32)
            rvc, RVC = k.sb("rvc", [128, 1], F32)
            t1s = [k.sb(f"t1{i}", [128, 512], F32) for i in range(2)]
            t2s = [k.sb(f"t2{i}", [128, 512], F32) for i in range(2)]
            qa, QA = k.sb("qa_st", [128, 8, 512], BF16, dma=True)
            qr, QR = k.sb("qr_st", [128, 4, 512], BF16, dma=True)
            ka, KA = k.sb("ka_st", [128, 8, 512], BF16, dma=True)
            kr, KR = k.sb("kr_st", [64, 512], BF16, dma=True)
            va, VA = k.sb("va_st", [128, 4, 1024], BF16, dma=True)
            ctr = [0]

            def nx():
                ctr[0] += 1
                return ctr[0]
            for bi, (t0, n) in enumerate(feature_blocks()):
                hT, HTB_ = hts[bi % 2]
                ta, TA = tas[bi % 2]
                k.dma(SP, hT[:, :, 0:n], fm(HTb[:, t0:t0 + n]), [], [HTB_], HTB_.d)
                k.dma(SP, ta[:, :, 0:n], tabA[:, :, t0:t0 + n].rearrange("c p t -> p c t"), [], [TA], TA.d)
                for c in range(4):
                    u = nx() % 2
                    mm(PT[u], ps[u][:, 0:n], [(wA[:, kc, c * 128:(c + 1) * 128], hT[:, kc, 0:n]) for kc in range(16)], [WA, HTB_])
                    sb_, SB_ = sqb[c % 2]
                    k.op(ACT, [PT[u]], [SB_], lambda e, u=u, sb_=sb_: e.activation(out=sb_[:, 0:n], in_=ps[u][:, 0:n], func=AF.Square))
                    k.op(ACT, [PT[u]] + CR, [CQG], lambda e, u=u, c=c: e.activation(out=cqg[:, c, 0:n], in_=ps[u][:, 0:n], func=AF.Identity, scale=cols[:, co + C_GQ + c:co + C_GQ + c + 1]))
                    mm(PT[2], ps[2][:, 0:n], [(ones_b[:, :], sb_[:, 0:n])], [SB_], start=(c == 0), stop=(c == 3))
                rsqrt_from(PT[2], ps[2][:, 0:n], RQ, rq[:, 0:n], 1.0 / 512, epsc[:, :])
                for c in range(2):
                    u = nx() % 2
                    mm(PT[u], ps[u][:, 0:n], [(wA[:, kc, 512 + c * 128:512 + (c + 1) * 128], hT[:, kc, 0:n]) for kc in range(16)], [WA, HTB_])
                    k.op(ACT, [PT[u]], [SQK], lambda e, u=u, c=c: e.activation(out=sqk[:, c, 0:n], in_=ps[u][:, 0:n], func=AF.Square))
                    k.op(ACT, [PT[u]] + CR, [CKG], lambda e, u=u, c=c: e.activation(out=ckg[:, c, 0:n], in_=ps[u][:, 0:n], func=AF.Identity, scale=cols[:, co + C_GKV + c:co + C_GKV + c + 1]))
                    mm(PT[3], ps[3][:, 0:n], [(ones_b[:, :], sqk[:, c, 0:n])], [SQK], start=(c == 0), stop=(c == 1))
                rsqrt_from(PT[3], ps[3][:, 0:n], RKV, rkv[:, 0:n], 1.0 / 256, epsc[:, :])
                for h in range(8):
                    u = nx() % 2
                    mm(PT[u], ps[u][:, 0:n], [(wq[:, kc, h * 128:(h + 1) * 128], cqg[:, kc, 0:n]) for kc in range(4)], [WA, CQG])
                    k.op(DVE, [PT[u], RQ], [QA], lambda e, u=u, h=h: e.tensor_tensor(out=qa[:, h, 0:n], in0=ps[u][:, 0:n], in1=rq[:, 0:n], op=ALU.mult))
                for j in range(4):
                    u = nx() % 2
                    mm(PT[u], ps[u][:, 0:n], [(wq[:, kc, 1024 + j * 128:1024 + (j + 1) * 128], cqg[:, kc, 0:n]) for kc in range(4)], [WA, CQG])
                    xr_, XR_ = xr[j % 2]
                    k.op(DVE, [PT[u], RQ], [XR_], lambda e, u=u, xr_=xr_: e.tensor_tensor(out=xr_[:, 0:n], in0=ps[u][:, 0:n], in1=rq[:, 0:n], op=ALU.mult))
                    t1, T1 = t1s[j % 2]
                    t2, T2 = t2s[j % 2]
                    rope_unit(128, n, xr_, XR_, 0, ta, TA, t1, T1, t2, T2, QR, qr[:, j, 0:n], 4 + j % 2)
                for h in range(8):
                    u = nx() % 2
                    mm(PT[u], ps[u][:, 0:n], [(wkv[:, kc, h * 128:(h + 1) * 128], ckg[:, kc, 0:n]) for kc in range(2)], [WA, CKG])
                    k.op(DVE, [PT[u], RKV], [KA], lambda e, u=u, h=h: e.tensor_tensor(out=ka[:, h, 0:n], in0=ps[u][:, 0:n], in1=rkv[:, 0:n], op=ALU.mult))
                u = nx() % 2
                mm(PT[u], ps[u][0:64, 0:n], [(wA[:, kc, 768:832], hT[:, kc, 0:n]) for kc in range(16)], [WA, HTB_])
                xr_, XR_ = xr[0]
                k.op(ACT, [PT[u]], [XR_], lambda e, u=u, xr_=xr_: e.activation(out=xr_[0:64, 0:n], in_=ps[u][0:64, 0:n], func=AF.Identity))
                rope_unit(64, n, xr_, XR_, 0, ta, TA, t1s[0][0], t1s[0][1], t2s[0][0], t2s[0][1], KR, kr[0:64, 0:n], 4)
                ntile = (n + 127) // 128
                for j in range(ntile):
                    p = min(128, n - j * 128)
                    mm(PT[6], ps[6][0:p, 0:1], [(sqk[:, c, j * 128:j * 128 + p], ones_b[:, 0:1]) for c in range(2)], [SQK])
                    k.op(ACT, [PT[6]] + CR, [RVC], lambda e: e.activation(out=rvc[0:p, :], in_=ps[6][0:p, 0:1], func=AF.Sqrt, bias=epsc[0:p, :], scale=1.0 / 256))
                    k.op(DVE, [RVC], [RVC], lambda e: e.reciprocal(out=rvc[0:p, :], in_=rvc[0:p, :]))
                    for hf in range(2):
                        u = nx() % 2
                        mm(PT[u], ps[u][0:p, :], [(ckg[:, c, j * 128:j * 128 + p], wkv[:, c, 1024 + hf * 512:1024 + (hf + 1) * 512]) for c in range(2)], [WA, CKG])
                        k.op(ACT, [PT[u], RVC], [VA], lambda e, u=u, hf=hf: e.activation(out=va[0:p, j, hf * 512:(hf + 1) * 512], in_=ps[u][0:p, :], func=AF.Identity, scale=rvc[0:p, 0:1]))
                k.dma(SP, QaT[:, :, t0:t0 + n].rearrange("h p t -> p h t"), qa[:, :, 0:n], [QA], [], QA.d)
                k.dma(SP, QrT[:, :, t0:t0 + n].rearrange("h p t -> p h t"), qr[:, :, 0:n], [QR], [], QR.d)
                k.dma(SP, KaT[:, :, t0:t0 + n].rearrange("h p t -> p h t"), ka[:, :, 0:n], [KA], [], KA.d)
                k.dma(SP, KrT[:, t0:t0 + n], kr[0:64, 0:n], [KR], [], KR.d)
                if n % 128 == 0:
                    k.dma(SP, Va[t0:t0 + n, :].rearrange("(j p) f -> p j f", p=128), va[:, 0:n // 128, :], [VA], [], VA.d)
                else:
                    k.dma(SP, Va[t0:t0 + n, :], va[0:n, 0, :], [VA], [], VA.d)
            k.end()

        def phaseA2(l):
            k.begin()
            co = l * NCOL
            wB, WB = k.sb("wB", [128, 16, 1536], BF16, dma="pool")
            k.dma(POOL, wB[:], fm(w_in[l, :, 832:2368]), [], [WB], WB.d)
            hts = [k.sb(f"hT{i}", [128, 16, 512], BF16, dma=True) for i in range(2)]
            tbs = [k.sb(f"tb{i}", [128, 2, 512], F32, dma=True) for i in range(2)]
            sqb = [k.sb(f"sqb{i}", [128, 512], BF16) for i in range(2)]
            xg = [k.sb(f"xg{i}", [128, 512], BF16) for i in range(2)]
            rrs = [k.sb(f"rr{i}", [128, 512], F32) for i in range(2)]
            t1s = [k.sb(f"t1{i}", [128, 512], F32) for i in range(2)]
            t2s = [k.sb(f"t2{i}", [128, 512], F32) for i in range(2)]
            gq, GQ = k.sb("gq_st", [128, 10, 512], BF16, dma=True)
            gv, GVS = k.sb("gv_st", [128, 4, 256], BF16, dma=True)
            for bi, (t0, n) in enumerate(feature_blocks()):
                hT, HTB_ = hts[bi % 2]
                tb, TBL = tbs[bi % 2]
                k.dma(SP, hT[:, :, 0:n], fm(HTb[:, t0:t0 + n]), [], [HTB_], HTB_.d)
                k.dma(SP, tb[:, :, 0:n], tabB[:, :, t0:t0 + n].rearrange("c p t -> p c t"), [], [TBL], TBL.d)
                for h in range(10):
                    u = h % 2
                    gcol = co + (C_GQQ if h < 8 else C_GQK)
                    mm(PT[u], ps[u][:, 0:n], [(wB[:, kc, h * 128:(h + 1) * 128], hT[:, kc, 0:n]) for kc in range(16)], [WB, HTB_])
                    sb_, SB_ = sqb[u]
                    xg_, XG_ = xg[u]
                    rr, RR = rrs[u]
                    k.op(ACT, [PT[u]], [SB_], lambda e, u=u, sb_=sb_: e.activation(out=sb_[:, 0:n], in_=ps[u][:, 0:n], func=AF.Square))
                    k.op(ACT, [PT[u]] + CR, [XG_], lambda e, u=u, xg_=xg_, gcol=gcol: e.activation(out=xg_[:, 0:n], in_=ps[u][:, 0:n], func=AF.Identity, scale=cols[:, gcol:gcol + 1]))
                    mm(PT[2 + u], ps[2 + u][:, 0:n], [(ones_b[:, :], sb_[:, 0:n])], [SB_])
                    rsqrt_from(PT[2 + u], ps[2 + u][:, 0:n], RR, rr[:, 0:n], 1.0 / 128, epsc[:, :])
                    rope_unit(128, n, xg_, XG_, 1, tb, TBL, t1s[u][0], t1s[u][1], t2s[u][0], t2s[u][1], GQ, gq[:, h, 0:n], 4 + u, rr, RR)
                ntile = (n + 127) // 128
                for j in range(ntile):
                    p = min(128, n - j * 128)
                    u = 6 + j % 2
                    mm(PT[u], ps[u][0:p, 0:256], [(hT[:, kc, j * 128:j * 128 + p], wB[:, kc, 1280:1536]) for kc in range(16)], [WB, HTB_])
                    k.op(ACT, [PT[u]], [GVS], lambda e, u=u: e.activation(out=gv[0:p, j, :], in_=ps[u][0:p, 0:256], func=AF.Identity))
                k.dma(SP, GQT[:, :, t0:t0 + n].rearrange("h p t -> p h t"), gq[:, 0:8, 0:n], [GQ], [], GQ.d)
                k.dma(SP, GKT[:, :, t0:t0 + n].rearrange("h p t -> p h t"), gq[:, 8:10, 0:n], [GQ], [], GQ.d)
                if n % 128 == 0:
                    k.dma(SP, GV[t0:t0 + n, :].rearrange("(j p) f -> p j f", p=128), gv[:, 0:n // 128, :], [GVS], [], GVS.d)
                else:
                    k.dma(SP, GV[t0:t0 + n, :], gv[0:n, 0, :], [GVS], [], GVS.d)
            k.end()

        def phaseB1(l):
            k.begin()
            kaS, KV = k.sb("kaS", [128, 8, T], BF16, dma=True)
            kr2, _ = k.sb("kr2", [128, T], BF16)
            gkS, _ = k.sb("gkS", [128, 2, T], BF16)
            vaS, _ = k.sb("vaS", [128, 17, 1024], BF16)
            gvS, _ = k.sb("gvS", [128, 17, 256], BF16)
            qsl = []
            for i in range(2):
                a_, A_ = k.sb(f"qa{i}", [128, 8, 512], BF16, dma=True)
                r_, _ = k.sb(f"qr{i}", [128, 4, 512], BF16)
                g_, _ = k.sb(f"gq{i}", [128, 8, 512], BF16)
                qsl.append((a_, r_, g_, A_))
            pts = [k.sb(f"pt{i}", [128, 512], BF16) for i in range(3)]
            rl, RL = k.sb("rl", [128, 512], F32)
            ots = [k.sb(f"ot{i}", [128, 512], F32, dma=True) for i in range(4)]
            qi = 0
            oi = 0
            hcount = 0
            for s in range(SPC):
                r0, m0 = s * SEQ, NR + s * NMETA
                for (dst, src) in [
                    (kaS[:, :, 0:SEQ], KaT[:, :, r0:r0 + SEQ].rearrange("h p t -> p h t")),
                    (kaS[:, :, SEQ:T], KaT[:, :, m0:m0 + NMETA].rearrange("h p t -> p h t")),
                    (kr2[0:64, 0:SEQ], KrT[:, r0:r0 + SEQ]), (kr2[64:128, 0:SEQ], KrT[:, r0:r0 + SEQ]),
                    (kr2[0:64, SEQ:T], KrT[:, m0:m0 + NMETA]), (kr2[64:128, SEQ:T], KrT[:, m0:m0 + NMETA]),
                    (gkS[:, :, 0:SEQ], GKT[:, :, r0:r0 + SEQ].rearrange("h p t -> p h t")),
                    (gkS[:, :, SEQ:T], GKT[:, :, m0:m0 + NMETA].rearrange("h p t -> p h t")),
                    (vaS[:, 0:16, :], Va[r0:r0 + SEQ, :].rearrange("(j p) f -> p j f", p=128)),
                    (vaS[0:16, 16, :], Va[m0:m0 + NMETA, :]),
                    (gvS[:, 0:16, :], GV[r0:r0 + SEQ, :].rearrange("(j p) f -> p j f", p=128)),
                    (gvS[0:16, 16, :], GV[m0:m0 + NMETA, :]),
                ]:
                    k.dma(SP, dst, src, [], [KV], KV.d)
                qblocks = [(r0 + i * 512, 512) for i in range(4)] + [(m0, NMETA)]
                for (t0, nq) in qblocks:
                    qa_, qr_, gq_, QS = qsl[qi % 2]
                    qi += 1
                    k.dma(SP, qa_[:, :, 0:nq], QaT[:, :, t0:t0 + nq].rearrange("h p t -> p h t"), [], [QS], QS.d)
                    k.dma(SP, qr_[:, :, 0:nq], QrT[:, :, t0:t0 + nq].rearrange("h p t -> p h t"), [], [QS], QS.d)
                    k.dma(SP, gq_[:, :, 0:nq], GQT[:, :, t0:t0 + nq].rearrange("h p t -> p h t"), [], [QS], QS.d)
                    for hh in range(16):
                        o = hcount % 2
                        hcount += 1
                        PO, PL = PT[3 + o], PT[5 + o]
                        pso, psl = ps[3 + o], ps[5 + o]

                        def emitS(kt, hh=hh):
                            k0 = kt * 128
                            kp = 128 if kt < 16 else NMETA
                            i = kt % 3
                            if hh < 8:
                                pb = 64 * (hh % 2)
                                pairs = [(kaS[:, hh, k0:k0 + kp], qa_[:, hh, 0:nq]),
                                         (kr2[pb:pb + 64, k0:k0 + kp], qr_[pb:pb + 64, hh // 2, 0:nq])]
                            else:
                                g = hh - 8
                                pairs = [(gkS[:, g // 4, k0:k0 + kp], gq_[:, g, 0:nq])]
                            mm(PT[i], ps[i][0:kp, 0:nq], pairs, [KV, QS])

                        def emitE(kt, hh=hh):
                            kp = 128 if kt < 16 else NMETA
                            i = kt % 3
                            pt_, PT_ = pts[i]
                            sc = MLA_SCALE if hh < 8 else GQA_SCALE
                            k.op(ACT, [PT[i]], [PT_], lambda e: e.activation(out=pt_[0:kp, 0:nq], in_=ps[i][0:kp, 0:nq], func=AF.Exp, scale=sc))

                        def emitO(kt, hh=hh):
                            kp = 128 if kt < 16 else NMETA
                            i = kt % 3
                            pt_, PT_ = pts[i]
                            if hh < 8:
                                vv = vaS[0:kp, kt, hh * 128:(hh + 1) * 128]
                            else:
                                g = (hh - 8) // 4
                                vv = gvS[0:kp, kt, g * 128:(g + 1) * 128]
                            mm(PO, pso[:, 0:nq], [(vv, pt_[0:kp, 0:nq])], [KV, PT_], start=(kt == 0), stop=(kt == 16))
                            mm(PL, psl[:, 0:nq], [(ones_b[0:kp, :], pt_[0:kp, 0:nq])], [PT_], start=(kt == 0), stop=(kt == 16))
                        emitS(0)
                        emitS(1)
                        for kt in range(17):
                            emitE(kt)
                            if kt + 2 < 17:
                                emitS(kt + 2)
                            emitO(kt)
                        ot_, OT_ = ots[oi % 4]
                        oi += 1
                        k.op(DVE, [PL], [RL], lambda e: e.reciprocal(out=rl[:, 0:nq], in_=psl[:, 0:nq]))
                        k.op(DVE, [PO, RL], [OT_], lambda e, ot_=ot_: e.tensor_tensor(out=ot_[:, 0:nq], in0=pso[:, 0:nq], in1=rl[:, 0:nq], op=ALU.mult))
                        k.dma(SP, OT[hh * 128:(hh + 1) * 128, t0:t0 + nq], ot_[:, 0:nq], [OT_], [], OT_.d)
            k.end()

        def ln_fm(n, z, Z, zb, ZB, gc, bc, eps_ap, tmps):
            (zsq, mean, MEAN, rstd, RSTD, nmr, NMR) = tmps
            mm(PT[6], ps[6][:, 0:n], [(ones_f[:, :], z(kc)) for kc in range(16)], [Z])
            for kc in range(16):
                zs, ZS = zsq[kc % 2]
                k.op(ACT, [Z], [ZS], lambda e, zs=zs, kc=kc: e.activation(out=zs[:, 0:n], in_=z(kc), func=AF.Square))
                mm(PT[7], ps[7][:, 0:n], [(ones_f[:, :], zs[:, 0:n])], [ZS], start=(kc == 0), stop=(kc == 15))
            k.op(ACT, [PT[6]], [MEAN], lambda e: e.activation(out=mean[:, 0:n], in_=ps[6][:, 0:n], func=AF.Identity, scale=1.0 / D))
            k.op(DVE, [MEAN], [NMR], lambda e: e.tensor_tensor(out=nmr[:, 0:n], in0=mean[:, 0:n], in1=mean[:, 0:n], op=ALU.mult))
            k.op(DVE, [PT[7], NMR], [RSTD], lambda e: e.scalar_tensor_tensor(out=rstd[:, 0:n], in0=ps[7][:, 0:n], scalar=1.0 / D, in1=nmr[:, 0:n], op0=ALU.mult, op1=ALU.subtract))
            k.op(ACT, [RSTD] + CR, [RSTD], lambda e: e.activation(out=rstd[:, 0:n], in_=rstd[:, 0:n], func=AF.Sqrt, bias=eps_ap, scale=1.0))
            k.op(DVE, [RSTD], [RSTD], lambda e: e.reciprocal(out=rstd[:, 0:n], in_=rstd[:, 0:n]))
            k.op(DVE, [MEAN, RSTD], [NMR], lambda e: e.tensor_tensor(out=nmr[:, 0:n], in0=mean[:, 0:n], in1=rstd[:, 0:n], op=ALU.mult))
            for kc in range(16):
                k.op(DVE, [Z, RSTD], [Z], lambda e, kc=kc: e.tensor_tensor(out=z(kc), in0=z(kc), in1=rstd[:, 0:n], op=ALU.mult))
                k.op(POOL, [Z, NMR], [Z], lambda e, kc=kc: e.tensor_tensor(out=z(kc), in0=z(kc), in1=nmr[:, 0:n], op=ALU.subtract))
                k.op(ACT, [Z] + CR, [Z], lambda e, kc=kc: e.activation(out=z(kc), in_=z(kc), func=AF.Identity, bias=cols[:, bc + kc:bc + kc + 1], scale=cols[:, gc + kc:gc + kc + 1]))
                if zb is not None:
                    k.op(POOL, [Z], [ZB], lambda e, kc=kc: e.tensor_copy(out=zb(kc), in_=z(kc)))

        def ln_tmps():
            zsq = [k.sb(f"zsq{i}", [128, 512], F32) for i in range(2)]
            mean, MEAN = k.sb("mean", [128, 512], F32)
            rstd, RSTD = k.sb("rstd", [128, 512], F32)
            nmr, NMR = k.sb("nmr", [128, 512], F32)
            return (zsq, mean, MEAN, rstd, RSTD, nmr, NMR)

        def phaseB2(l):
            k.begin()
            co = l * NCOL
            wo, WO = k.sb("wo", [128, 16, D], BF16, dma="pool")
            k.dma(POOL, wo[:], fm(w_out[l]), [], [WO], WO.d)
            ot, OTB = k.sb("ot", [128, 16, 512], F32, dma=True)
            onb, ONB = k.sb("onb", [128, 16, 512], BF16)
            hz, HZ = k.sb("hz", [128, 16, 512], F32, dma=True)
            hzb, HZB = k.sb("hzb", [128, 16, 512], BF16, dma=True)
            sqb = [k.sb(f"sqb{i}", [128, 512], BF16) for i in range(2)]
            ra, RA = k.sb("ra", [128, 512], F32)
            rb, RB = k.sb("rb", [128, 512], F32)
            tmps = ln_tmps()
            for (t0, n) in feature_blocks():
                k.dma(SP, ot[:, :, 0:n], fm(OT[:, t0:t0 + n]), [], [OTB], OTB.d)
                k.dma(SP, hz[:, :, 0:n], fm(HT32[:, t0:t0 + n]), [], [HZ], HZ.d)
                for grp in range(2):
                    for c in range(8):
                        kc = grp * 8 + c
                        sb_, SB_ = sqb[kc % 2]
                        k.op(ACT, [OTB], [SB_], lambda e, sb_=sb_, kc=kc: e.activation(out=sb_[:, 0:n], in_=ot[:, kc, 0:n], func=AF.Square))
                        mm(PT[2 + grp], ps[2 + grp][:, 0:n], [(ones_b[:, :], sb_[:, 0:n])], [SB_], start=(c == 0), stop=(c == 7))
                rsqrt_from(PT[2], ps[2][:, 0:n], RA, ra[:, 0:n], 1.0 / 1024, epsc[:, :])
                rsqrt_from(PT[3], ps[3][:, 0:n], RB, rb[:, 0:n], 1.0 / 1024, epsc[:, :])
                for kc in range(16):
                    r_, R_ = (ra, RA) if kc < 8 else (rb, RB)
                    gcx = co + C_GOA + kc
                    k.op(DVE, [OTB, R_] + CR, [ONB], lambda e, kc=kc, r_=r_, gcx=gcx: e.scalar_tensor_tensor(out=onb[:, kc, 0:n], in0=ot[:, kc, 0:n], scalar=cols[:, gcx:gcx + 1], in1=r_[:, 0:n], op0=ALU.mult, op1=ALU.mult))
                for fc in range(16):
                    u = fc % 2
                    mm(PT[u], ps[u][:, 0:n], [(wo[:, kc, fc * 128:(fc + 1) * 128], onb[:, kc, 0:n]) for kc in range(16)], [WO, ONB])
                    k.op(DVE, [PT[u], HZ], [HZ], lambda e, u=u, fc=fc: e.scalar_tensor_tensor(out=hz[:, fc, 0:n], in0=hz[:, fc, 0:n], scalar=ALPHA, in1=ps[u][:, 0:n], op0=ALU.mult, op1=ALU.add))
                ln_fm(n, lambda kc: hz[:, kc, 0:n], HZ, lambda kc: hzb[:, kc, 0:n], HZB, co + C_L1G, co + C_L1B, epsc[:, :], tmps)
                k.dma(SP, fm(HT32[:, t0:t0 + n]), hz[:, :, 0:n], [HZ], [], HZ.d)
                k.dma(SP, fm(HTb[:, t0:t0 + n]), hzb[:, :, 0:n], [HZB], [], HZB.d)
            k.end()

        def phaseC(l, last):
            k.begin()
            co = l * NCOL
            TBM = 1024 + NM
            hT, HT_ = k.sb("h1T", [128, 16, TBM], BF16, dma=True)
            ya, YA = k.sb("yacc", [128, 16, TBM], F32, dma=True)
            he, HE = k.sb("heT", [128, 8, TBM], BF16)
            cbs = [k.sb(f"cbc{i}", [128, TBM], BF16) for i in range(2)]
            wg = [k.sb(f"wg{i}", [128, 16, 256], BF16, dma="pool") for i in range(2)]
            wu = [k.sb(f"wu{i}", [128, 16, 256], BF16, dma="pool") for i in range(2)]
            wd = [k.sb(f"wd{i}", [128, 8, 512], BF16, dma="pool") for i in range(2)]
            sgs = [k.sb(f"sg{i}", [128, 512], BF16) for i in range(2)]
            tts = [k.sb(f"tt{i}", [128, 512], BF16) for i in range(2)]
            cmbT, CMBT = k.sb("cmbT", [NE, TBM], F32)
            rt, RT = k.sb("rt", [128, 12, NE], F32)
            rs, RS = k.sb("rs", [128, 8], F32)
            tmps = ln_tmps()
            if last:
                osts = [k.sb(f"ost{i}", [128, 1024], F32, dma=True) for i in range(1)]
            nblk = NR // 1024
            wcnt = [0, 0]
            for b in range(nblk):
                subs = [(b * 1024, 512, 0), (b * 1024 + 512, 512, 512)]
                if b == nblk - 1:
                    subs.append((NR, NM, 1024))
                for (t0, n, c0) in subs:
                    k.dma(SP, hT[:, :, c0:c0 + n], fm(HTb[:, t0:t0 + n]), [], [HT_], HT_.d)
                    k.dma(SP, ya[:, :, c0:c0 + n], fm(HT32[:, t0:t0 + n]), [], [YA], YA.d)
                for (t0, n, c0) in subs:
                    for j in range((n + 127) // 128):
                        p = min(128, n - j * 128)
                        cc = c0 + j * 128
                        mm(PT[0], ps[0][0:p, 0:NE], [(ya[:, kc, cc:cc + p], wrt[:, kc, :]) for kc in range(16)], [YA])
                        S_, SEL_, M1, EQ, S2, M2, MSK, IS1, IS2 = [rt[0:p, i, :] for i in range(9)]
                        k.op(ACT, [PT[0]], [RT], lambda e: e.activation(out=S_, in_=ps[0][0:p, 0:NE], func=AF.Sigmoid))
                        k.op(DVE, [RT] + CR, [RT], lambda e: e.tensor_tensor(out=SEL_, in0=S_, in1=rb_bc[0:p, :], op=ALU.add))
                        sel3 = SEL_.rearrange("p (g e) -> p g e", e=4)
                        k.op(DVE, [RT], [RS], lambda e: e.tensor_reduce(out=rs[0:p, 0:4], in_=sel3, axis=AX.X, op=ALU.max))
                        k.op(DVE, [RT, RS], [RT], lambda e: e.tensor_tensor(out=EQ.rearrange("p (g e) -> p g e", e=4), in0=sel3, in1=rs[0:p, 0:4].unsqueeze(2).to_broadcast([p, 4, 4]), op=ALU.is_equal))
                        k.op(DVE, [RT], [RT], lambda e: e.scalar_tensor_tensor(out=S2, in0=EQ, scalar=NEG, in1=SEL_, op0=ALU.mult, op1=ALU.add))
                        k.op(DVE, [RT], [RS], lambda e: e.tensor_reduce(out=rs[0:p, 4:8], in_=S2.rearrange("p (g e) -> p g e", e=4), axis=AX.X, op=ALU.max))
                        k.op(DVE, [RS], [RS], lambda e: e.tensor_tensor(out=rs[0:p, 0:4], in0=rs[0:p, 0:4], in1=rs[0:p, 4:8], op=ALU.add))
                        k.op(DVE, [RS], [RS], lambda e: e.tensor_reduce(out=rs[0:p, 4:5], in_=rs[0:p, 0:4], axis=AX.X, op=ALU.max))
                        k.op(DVE, [RS], [RS], lambda e: e.tensor_scalar(out=rs[0:p, 0:4], in0=rs[0:p, 0:4], scalar1=rs[0:p, 4:5], scalar2=None, op0=ALU.is_equal))
                        k.op(DVE, [RS], [RS], lambda e: e.tensor_scalar(out=rs[0:p, 0:4], in0=rs[0:p, 0:4], scalar1=-1.0, scalar2=-NEG, op0=ALU.add, op1=ALU.mult))
                        k.op(DVE, [RT, RS], [RT], lambda e: e.tensor_tensor(out=MSK.rearrange("p (g e) -> p g e", e=4), in0=sel3, in1=rs[0:p, 0:4].unsqueeze(2).to_broadcast([p, 4, 4]), op=ALU.add))
                        k.op(DVE, [RT], [RS], lambda e: e.tensor_reduce(out=rs[0:p, 5:6], in_=MSK, axis=AX.X, op=ALU.max))
                        k.op(DVE, [RT, RS], [RT], lambda e: e.tensor_scalar(out=IS1, in0=MSK, scalar1=rs[0:p, 5:6], scalar2=None, op0=ALU.is_equal))
                        k.op(DVE, [RT], [RT], lambda e: e.scalar_tensor_tensor(out=MSK, in0=IS1, scalar=NEG, in1=MSK, op0=ALU.mult, op1=ALU.add))
                        k.op(DVE, [RT], [RS], lambda e: e.tensor_reduce(out=rs[0:p, 6:7], in_=MSK, axis=AX.X, op=ALU.max))
                        k.op(DVE, [RT, RS], [RT], lambda e: e.tensor_scalar(out=IS2, in0=MSK, scalar1=rs[0:p, 6:7], scalar2=None, op0=ALU.is_equal))
                        k.op(DVE, [RT], [RT], lambda e: e.tensor_tensor(out=IS1, in0=IS1, in1=IS2, op=ALU.add))
                        k.op(DVE, [RT], [RT], lambda e: e.tensor_tensor(out=IS1, in0=IS1, in1=S_, op=ALU.mult))
                        k.op(DVE, [RT], [RS], lambda e: e.tensor_reduce(out=rs[0:p, 7:8], in_=IS1, axis=AX.X, op=ALU.add))
                        k.op(DVE, [RS], [RS], lambda e: e.reciprocal(out=rs[0:p, 7:8], in_=rs[0:p, 7:8]))
                        k.op(DVE, [RT, RS], [RT], lambda e: e.tensor_scalar(out=IS2, in0=IS1, scalar1=rs[0:p, 7:8], scalar2=1.0 / ALPHA, op0=ALU.mult, op1=ALU.mult))
                        k.op(PE, [RT] + CR, [PT[1]], lambda e: e.transpose(ps[1][0:NE, 0:p], IS2, ident[0:p, 0:p]))
                        k.op(ACT, [PT[1]], [CMBT], lambda e: e.activation(out=cmbT[:, cc:cc + p], in_=ps[1][0:NE, 0:p], func=AF.Identity))
                def load_gu(i):
                    if i >= NE * 4:
                        return
                    ex_, q_ = divmod(i, 4)
                    g_, G_ = wg[i % 2]
                    u_, U_ = wu[i % 2]
                    k.dma(POOL, g_[:], fm(w_gate[l, ex_, :, q_ * 256:(q_ + 1) * 256]), [], [G_], G_.d)
                    k.dma(POOL, u_[:], fm(w_up[l, ex_, :, q_ * 256:(q_ + 1) * 256]), [], [U_], U_.d)

                def load_d(i):
                    if i >= NE * 4:
                        return
                    ex_, q_ = divmod(i, 4)
                    d_, D_ = wd[i % 2]
                    k.dma(POOL, d_[:], w_down[l, ex_, :, q_ * 512:(q_ + 1) * 512].rearrange("(fc p) f -> p fc f", p=128), [], [D_], D_.d)
                for ex in range(NE):
                    cb, CB = cbs[ex % 2]
                    for si, (t0, n, c0) in enumerate(subs):
                        u = 6 + si % 2
                        mm(PT[u], ps[u][:, 0:n], [(sel[:, ex * 128:(ex + 1) * 128], cmbT[:, c0:c0 + n])], [CMBT])
                        k.op(ACT, [PT[u]], [CB], lambda e, u=u, c0=c0, n=n, cb=cb: e.activation(out=cb[:, c0:c0 + n], in_=ps[u][:, 0:n], func=AF.Identity))
                    for q4 in range(4):
                        if ex == 0 and q4 == 0:
                            load_gu(0)
                        load_gu(ex * 4 + q4 + 1)
                        g_, G_ = wg[(ex * 4 + q4) % 2]
                        u_, U_ = wu[(ex * 4 + q4) % 2]
                        for f2 in range(2):
                            ffc = q4 * 2 + f2
                            for si, (t0, n, c0) in enumerate(subs):
                                pg, pu = si % 2, 2 + si % 2
                                mm(PT[pg], ps[pg][:, 0:n], [(g_[:, kc, f2 * 128:(f2 + 1) * 128], hT[:, kc, c0:c0 + n]) for kc in range(16)], [G_, HT_])
                                mm(PT[pu], ps[pu][:, 0:n], [(u_[:, kc, f2 * 128:(f2 + 1) * 128], hT[:, kc, c0:c0 + n]) for kc in range(16)], [U_, HT_])
                                sg, SG = sgs[si % 2]
                                tt, TT = tts[si % 2]
                                k.op(ACT, [PT[pg]], [SG], lambda e, pg=pg, sg=sg, n=n: e.activation(out=sg[:, 0:n], in_=ps[pg][:, 0:n], func=AF.Silu))
                                k.op(DVE, [PT[pu], CB], [TT], lambda e, pu=pu, tt=tt, n=n, c0=c0, cb=cb: e.tensor_tensor(out=tt[:, 0:n], in0=ps[pu][:, 0:n], in1=cb[:, c0:c0 + n], op=ALU.mult))
                                k.op(POOL, [SG, TT], [HE], lambda e, sg=sg, tt=tt, n=n, c0=c0, ffc=ffc: e.tensor_tensor(out=he[:, ffc, c0:c0 + n], in0=sg[:, 0:n], in1=tt[:, 0:n], op=ALU.mult))
                    for q4 in range(4):
                        if ex == 0 and q4 == 0:
                            load_d(0)
                        load_d(ex * 4 + q4 + 1)
                        d_, D_ = wd[(ex * 4 + q4) % 2]
                        for f4 in range(4):
                            fc = q4 * 4 + f4
                            for si, (t0, n, c0) in enumerate(subs):
                                pd = 4 + (f4 * 3 + si) % 2
                                mm(PT[pd], ps[pd][:, 0:n], [(d_[:, ffc, f4 * 128:(f4 + 1) * 128], he[:, ffc, c0:c0 + n]) for ffc in range(8)], [D_, HE])
                                k.op(DVE, [PT[pd], YA], [YA], lambda e, pd=pd, fc=fc, n=n, c0=c0: e.tensor_tensor(out=ya[:, fc, c0:c0 + n], in0=ya[:, fc, c0:c0 + n], in1=ps[pd][:, 0:n], op=ALU.add))
                for (t0, n, c0) in subs:
                    ln_fm(n, lambda kc, c0=c0, n=n: ya[:, kc, c0:c0 + n], YA,
                          (None if last else (lambda kc, c0=c0, n=n: hT[:, kc, c0:c0 + n])), HT_,
                          co + C_L2G, co + C_L2B, eps2c[:, :], tmps)
                    if not last:
                        k.dma(SP, fm(HT32[:, t0:t0 + n]), ya[:, :, c0:c0 + n], [YA], [], YA.d)
                        k.dma(SP, fm(HTb[:, t0:t0 + n]), hT[:, :, c0:c0 + n], [HT_], [], HT_.d)
                    elif t0 < NR:
                        for j in range(n // 128):
                            os_, OS_ = osts[0]
                            s, s0 = divmod(t0 + j * 128, SEQ)
                            for b4 in range(4):
                                def tr(e, b4=b4, j=j, c0=c0):
                                    ins = None
                                    for q in range(4):
                                        kc = b4 * 4 + q
                                        ins = e.transpose(ps[b4][:, q * 128:(q + 1) * 128], ya[:, kc, c0 + j * 128:c0 + (j + 1) * 128], ident[:, :])
                                    return ins
                                k.op(PE, [YA] + CR, [PT[b4]], tr)
                                hb4 = b4 % 2
                                if b4 % 2 == 0:
                                    k.op(ACT, [PT[b4]], [OS_], lambda e, b4=b4, os_=os_, hb4=hb4: e.activation(out=os_[:, hb4 * 512:(hb4 + 1) * 512], in_=ps[b4][:, :], func=AF.Identity))
                                else:
                                    k.op(DVE, [PT[b4]], [OS_], lambda e, b4=b4, os_=os_, hb4=hb4: e.tensor_copy(out=os_[:, hb4 * 512:(hb4 + 1) * 512], in_=ps[b4][:, :]))
                                    hf = b4 // 2
                                    k.dma(SP, out[s, s0:s0 + 128, hf * 1024:(hf + 1) * 1024], os_[:, :], [OS_], [], OS_.d)
            k.end()

        stop = dbg if isinstance(dbg, str) else None
        if stop == "const":
            k.barrier()
            return nc
        phase0()
        for l in range(depth):
            if stop == "p0":
                break
            phaseA1(l)
            phaseA2(l)
            if stop == "a":
                break
            phaseB1(l)
            if stop == "b1":
                break
            phaseB2(l)
            if stop == "b2":
                break
            phaseC(l, l == depth - 1)
    return nc


def _rope_tables(SPC):
    NR, NM = SPC * SEQ, SPC * NMETA
    ROWS = SEQ // 64
    pr = np.concatenate([np.tile(np.repeat(np.arange(ROWS, dtype=np.float32), 64), SPC), np.full((NM,), -1.0, np.float32)])
    pc = np.concatenate([np.tile(np.tile(np.arange(64, dtype=np.float32), ROWS), SPC), np.tile(np.arange(NMETA, dtype=np.float32), SPC)])

    def tab(rot):
        n = rot // 4
        inv = (np.float32(10000.0) ** (-np.arange(n, dtype=np.float32) / np.float32(n))).astype(np.float32)
        ang = np.concatenate([pr[:, None] * inv, pc[:, None] * inv], axis=-1).astype(np.float32)
        c, s = np.cos(ang).T.astype(np.float32), np.sin(ang).T.astype(np.float32)
        reps = 128 // (rot // 2)
        return np.ascontiguousarray(np.stack([np.concatenate([c] * reps, 0), np.concatenate([s] * reps, 0)], 0))
    return tab(64), tab(128)


def _rmats():
    def R(half):
        m = np.zeros((2 * half, 2 * half), np.float32)
        for i in range(half):
            m[i + half, i] = -1.0
            m[i, i + half] = 1.0
        return m
    ra = np.zeros((128, 128), np.float32)
    ra[0:64, 0:64] = R(32)
    ra[64:128, 64:128] = R(32)
    return np.stack([ra, R(64)], 0)


def _prep_shared(inp, depth):
    f = lambda a: np.ascontiguousarray(np.asarray(a, dtype=np.float32))
    qperm = np.concatenate([np.arange(h * 192, h * 192 + 128) for h in range(8)] + [np.arange(h * 192 + 128, h * 192 + 192) for h in range(8)])
    kperm = np.concatenate([np.arange(h * 256, h * 256 + 128) for h in range(8)] + [np.arange(h * 256 + 128, h * 256 + 256) for h in range(8)])
    cols = np.zeros((128, depth * NCOL), np.float32)

    def put(l, off, v):
        v = np.asarray(v, np.float32)
        c = v.shape[0] // 128
        cols[:, l * NCOL + off:l * NCOL + off + c] = v.reshape(c, 128).T
    for l in range(depth):
        put(l, C_GQ, inp["g_q_lora"][l]); put(l, C_GKV, inp["g_kv_lora"][l])
        put(l, C_GQQ, inp["g_qk_q"][l]); put(l, C_GQK, inp["g_qk_k"][l])
        put(l, C_GOA, inp["g_out_mla"][l]); put(l, C_GOB, inp["g_out_gqa"][l])
        put(l, C_L1G, inp["ln1_g"][l]); put(l, C_L1B, inp["ln1_b"][l])
        put(l, C_L2G, inp["ln2_g"][l]); put(l, C_L2B, inp["ln2_b"][l])
    sel = np.zeros((NE, NE, 128), np.float32)
    for e in range(NE):
        sel[e, e, :] = 1.0
    return {
        "meta": f(inp["meta_tokens"]), "lng": f(inp["ln_in_g"]), "lnb": f(inp["ln_in_b"]),
        "w_in": f(inp["w_in"][:depth]), "w_qb": f(np.asarray(inp["w_q_b"])[:depth][:, :, qperm]),
        "w_kvb": f(np.asarray(inp["w_kv_b"])[:depth][:, :, kperm]), "w_out": f(inp["w_out"][:depth]),
        "w_rt": f(inp["w_router"]), "rbias": f(inp["router_bias"]),
        "w_gate": f(inp["w_gate"][:depth]), "w_up": f(inp["w_up"][:depth]), "w_down": f(inp["w_down"][:depth]),
        "cols": cols, "ident": np.eye(128, dtype=np.float32), "rmat": _rmats(), "sel": sel.reshape(NE, NE * 128),
    }


def kernel(**inputs):
    SPC = SPC_DEFAULT
    ncores = 16 // SPC
    x = np.asarray(inputs["x"], dtype=np.float32)
    shared = _prep_shared(inputs, DEPTH)
    tA, tB = _rope_tables(SPC)
    shared["tabA"], shared["tabB"] = tA, tB
    nc = build_nc(SPC, DEPTH)
    in_maps = []
    for c in range(ncores):
        m = dict(shared)
        m["x"] = np.ascontiguousarray(x[c * SPC:(c + 1) * SPC])
        in_maps.append(m)
    res = run_bass_kernel_spmd(nc, in_maps, core_ids=list(range(ncores)))
    return np.concatenate([r["out"] for r in res.results], axis=0).astype(np.float32)
```

```python
import math
from contextlib import ExitStack
import numpy as np
import concourse.bass as bass
import concourse.mybir as mybir
from concourse.bass_utils import run_bass_kernel_spmd

F32 = mybir.dt.float32
BF16 = mybir.dt.bfloat16
AF = mybir.ActivationFunctionType
ALU = mybir.AluOpType
AX = mybir.AxisListType

D = 2048
SEQ = 2048
NMETA = 16
T = SEQ + NMETA
DEPTH = 4
INC = 2368
EPS = 1e-6
ALPHA = (2.0 * DEPTH) ** 0.25
MLA_SCALE = 1.0 / math.sqrt(192.0)
GQA_SCALE = 1.0 / math.sqrt(128.0)
NE = 16
FF = 1024
NCOL = 88
C_GQ, C_GKV, C_GQQ, C_GQK, C_GOA, C_GOB, C_L1G, C_L1B, C_L2G, C_L2B = 0, 4, 6, 7, 8, 16, 24, 40, 56, 72
NEG = -1.0e30

SPC_DEFAULT = 2


class St:
    def __init__(self, eng, sem, own=True):
        self.eng, self.sem, self.n, self.seen, self.own = eng, sem, 0, {}, own


class DSem:
    def __init__(self, sem):
        self.sem, self.cnt = sem, 0


class TB:
    def __init__(self, name, d=None):
        self.name, self.w, self.r, self.d = name, None, {}, d


class K:
    def __init__(self, nc, es):
        self.nc = nc
        sems = [es.enter_context(nc.semaphore(f"s{i}")) for i in range(40)]
        self.PE = St(nc.tensor, sems[0], own=False)
        self.ACT = St(nc.scalar, sems[1])
        self.DVE = St(nc.vector, sems[2])
        self.POOL = St(nc.gpsimd, sems[3])
        self.SP = St(nc.sync, sems[4])
        self.streams = [self.PE, self.ACT, self.DVE, self.POOL, self.SP]
        self.dpool = [DSem(s) for s in sems[5:30]]
        self.ppool = [DSem(s) for s in sems[30:]]
        self.dnext = 0
        self.pnext = 0
        self.pes = None

    def _wait(self, st, ev):
        if ev is None:
            return
        if ev[0] == "d":
            sem, val = ev[1].sem, 16 * ev[1].cnt
        else:
            sem, val = ev[1], ev[2]
            if sem is st.sem and not st.own:
                return
        if st.seen.get(sem.num, 0) >= val:
            return
        st.eng.wait_ge(sem, val)
        st.seen[sem.num] = val

    def dep(self, st, reads, writes):
        for b in reads:
            self._wait(st, b.w)
        for b in writes:
            self._wait(st, b.w)
            for ev in list(b.r.values()):
                self._wait(st, ev)

    def fin(self, ev, key, reads, writes):
        for b in reads:
            b.r[key] = ev
        for b in writes:
            b.w = ev
            b.r = {}

    def op(self, st, reads, writes, fn):
        self.dep(st, reads, writes)
        ins = fn(st.eng)
        st.n += 1
        ins.then_inc(st.sem, 1)
        self.fin(("c", st.sem, st.n), st.sem.num, reads, writes)

    def dma(self, q, out, in_, reads, writes, d):
        self.dep(q, reads, writes)
        ins = q.eng.dma_start(out=out, in_=in_)
        d.cnt += 1
        ins.then_inc(d.sem, 16)
        self.fin(("d", d), d.sem.num, reads, writes)

    def begin(self):
        self.pes = ExitStack()
        self.dnext = 0
        self.pnext = 0

    def sb(self, name, shape, dt, dma=False):
        self.uid = getattr(self, "uid", 0) + 1
        t = self.pes.enter_context(self.nc.sbuf_tensor(f"{name}_u{self.uid}", shape, dt))
        d = None
        if dma == "pool":
            d = self.ppool[self.pnext]
            self.pnext += 1
        elif dma:
            d = self.dpool[self.dnext]
            self.dnext += 1
        return t, TB(name, d)

    def barrier(self):
        evs = [("c", s.sem, s.n) for s in self.streams if s.n > 0]
        evs += [("d", d) for d in self.dpool + self.ppool if d.cnt > 0]
        for s in self.streams:
            for ev in evs:
                self._wait(s, ev)

    def end(self):
        self.barrier()
        self.pes.close()
        self.pes = None


def build_nc(SPC=SPC_DEFAULT, depth=DEPTH, dbg=False):
    nc = bass.Bass("TRN2", target_bir_lowering=False)
    NR = SPC * SEQ
    NM = SPC * NMETA
    NT = NR + NM

    def din(name, shape, dt=F32):
        return nc.dram_tensor(name, list(shape), dt, kind="ExternalInput").ap()

    x = din("x", [SPC, SEQ, D])
    meta = din("meta", [NMETA, D])
    lng = din("lng", [D])
    lnb = din("lnb", [D])
    w_in = din("w_in", [depth, D, INC])
    w_qb = din("w_qb", [depth, 512, 1536])
    w_kvb = din("w_kvb", [depth, 256, 2048])
    w_out = din("w_out", [depth, D, D])
    w_rt = din("w_rt", [D, NE])
    rbias = din("rbias", [NE])
    w_gate = din("w_gate", [depth, NE, D, FF])
    w_up = din("w_up", [depth, NE, D, FF])
    w_down = din("w_down", [depth, NE, FF, D])
    cols_d = din("cols", [128, depth * NCOL])
    ident_d = din("ident", [128, 128])
    rmat_d = din("rmat", [2, 128, 128])
    sel_d = din("sel", [NE, NE * 128])
    tabA = din("tabA", [2, 128, NT])
    tabB = din("tabB", [2, 128, NT])
    out = nc.dram_tensor("out", [SPC, SEQ, D], F32, kind="ExternalOutput").ap()

    okind = "ExternalOutput" if dbg else "Internal"

    def dscr(name, shape, dt):
        return nc.dram_tensor(name, list(shape), dt, kind=okind).ap()

    HT32 = dscr("HT32", [D, NT], F32)
    HTb = dscr("HTb", [D, NT], BF16)
    QaT = dscr("QaT", [8, 128, NT], BF16)
    QrT = dscr("QrT", [4, 128, NT], BF16)
    KaT = dscr("KaT", [8, 128, NT], BF16)
    KrT = dscr("KrT", [64, NT], BF16)
    Va = dscr("Va", [NT, 1024], BF16)
    GQT = dscr("GQT", [8, 128, NT], BF16)
    GKT = dscr("GKT", [2, 128, NT], BF16)
    GV = dscr("GV", [NT, 256], BF16)
    OT = dscr("OT", [D, NT], F32)

    def fm(ap2d):
        return ap2d.rearrange("(kc p) t -> p kc t", p=128)

    with ExitStack() as es:
        k = K(nc, es)
        PE, ACT, DVE, POOL, SP = k.PE, k.ACT, k.DVE, k.POOL, k.SP
        ps = [es.enter_context(nc.psum_tensor(f"ps{i}", [128, 512], F32)) for i in range(8)]
        PT = [TB(f"ps{i}") for i in range(8)]

        def gsb(name, shape, dt):
            return es.enter_context(nc.sbuf_tensor("g_" + name, shape, dt))

        gd = k.dpool.pop()
        ident = gsb("ident", [128, 128], F32)
        ones_f = gsb("ones_f", [128, 128], F32)
        ones_b = gsb("ones_b", [128, 128], BF16)
        rmat = gsb("rmat", [128, 2, 128], BF16)
        epsc = gsb("epsc", [128, 1], F32)
        eps2c = gsb("eps2c", [128, 1], F32)
        sel = gsb("sel", [NE, NE * 128], F32)
        cols = gsb("cols", [128, depth * NCOL], F32)
        rb_bc = gsb("rb_bc", [128, NE], F32)
        wrt = gsb("wrt", [128, 16, NE], F32)
        CONST = TB("const", gd)
        k.dma(SP, ident[:], ident_d[:, :], [], [CONST], gd)
        k.dma(SP, sel[:], sel_d[:, :], [], [CONST], gd)
        k.dma(SP, cols[:], cols_d[:, :], [], [CONST], gd)
        k.dma(SP, rb_bc[:], rbias.partition_broadcast(128), [], [CONST], gd)
        k.dma(SP, wrt[:], w_rt.rearrange("(kc p) e -> p kc e", p=128), [], [CONST], gd)
        gdp = k.ppool.pop()
        CONSTP = TB("constp", gdp)
        k.dma(POOL, rmat[:], rmat_d.rearrange("c p m -> p c m"), [], [CONSTP], gdp)
        CM = TB("constmem")
        k.op(DVE, [], [CM], lambda e: e.memset(ones_f[:], 1.0))
        k.op(DVE, [], [CM], lambda e: e.memset(ones_b[:], 1.0))
        k.op(DVE, [], [CM], lambda e: e.memset(epsc[:], EPS))
        k.op(DVE, [], [CM], lambda e: e.memset(eps2c[:], EPS / (ALPHA * ALPHA)))
        CR = [CONST, CONSTP, CM]

        def mm(pt, out_ap, pairs, reads, start=True, stop=True):
            def f(e):
                ins = None
                n = len(pairs)
                for i, (l, r) in enumerate(pairs):
                    ins = e.matmul(out_ap, l, r, start=(start and i == 0), stop=(stop and i == n - 1))
                return ins
            k.op(PE, reads + CR, [pt], f)

        def feature_blocks():
            bl = [(i * 512, 512) for i in range(NR // 512)]
            bl.append((NR, NM))
            return bl

        def rsqrt_from(pt, ps_ap, out_tb, out_ap, scale, eps_ap):
            k.op(ACT, [pt] + CR, [out_tb], lambda e: e.activation(out=out_ap, in_=ps_ap, func=AF.Sqrt, bias=eps_ap, scale=scale))
            k.op(DVE, [out_tb], [out_tb], lambda e: e.reciprocal(out=out_ap, in_=out_ap))

        def phase0():
            k.begin()
            xt = [k.sb(f"xt{i}", [128, D], F32, dma=True) for i in range(2)]
            hb, HB = k.sb("hb", [128, D], F32)
            gbc, GB = k.sb("gbc", [128, D], F32, dma=True)
            bbc, BB = k.sb("bbc", [128, D], F32, dma=True)
            stf, STF = k.sb("stf", [128, 16, 512], F32, dma=True)
            stb, STB = k.sb("stb", [128, 16, 512], BF16, dma=True)
            sm, SM = k.sb("sm", [128, 8], F32)
            k.dma(SP, gbc[:], lng.partition_broadcast(128), [], [GB], GB.d)
            k.dma(SP, bbc[:], lnb.partition_broadcast(128), [], [BB], BB.d)
            it = 0
            import os as _os
            for (t0, n) in feature_blocks():
                if _os.environ.get("P0_SKIP_META") and n < 128:
                    continue
                ntile = (n + 127) // 128
                for j in range(ntile):
                    p = min(128, n - j * 128)
                    xs, XS = xt[it % 2]
                    it += 1
                    if t0 < NR:
                        s, s0 = divmod(t0 + j * 128, SEQ)
                        k.dma(SP, xs[0:p, :], x[s, s0:s0 + p, :], [], [XS], XS.d)
                    else:
                        for s in range(SPC):
                            k.dma(SP, xs[16 * s:16 * s + 16, :], meta[:, :], [], [XS], XS.d)
                    _step = int(_os.environ.get("P0_STEP", "99"))
                    if _step <= 1:
                        k.end(); return
                    k.op(DVE, [], [SM], lambda e: e.memset(sm[0:p, 0:2], 0.0))
                    k.op(ACT, [XS], [HB, SM], lambda e: e.activation(out=hb[0:p, :], in_=xs[0:p, :], func=AF.Identity, accum_out=sm[0:p, 0:1]))
                    k.op(ACT, [XS], [HB, SM], lambda e: e.activation(out=hb[0:p, :], in_=xs[0:p, :], func=AF.Square, accum_out=sm[0:p, 1:2]))
                    if _step <= 2:
                        k.end(); return
                    k.op(DVE, [SM], [SM], lambda e: e.tensor_scalar(out=sm[0:p, 2:3], in0=sm[0:p, 0:1], scalar1=1.0 / D, scalar2=None, op0=ALU.mult))
                    k.op(DVE, [SM], [SM], lambda e: e.tensor_tensor(out=sm[0:p, 3:4], in0=sm[0:p, 2:3], in1=sm[0:p, 2:3], op=ALU.mult))
                    k.op(DVE, [SM], [SM], lambda e: e.scalar_tensor_tensor(out=sm[0:p, 4:5], in0=sm[0:p, 1:2], scalar=1.0 / D, in1=sm[0:p, 3:4], op0=ALU.mult, op1=ALU.subtract))
                    k.op(ACT, [SM] + CR, [SM], lambda e: e.activation(out=sm[0:p, 5:6], in_=sm[0:p, 4:5], func=AF.Sqrt, bias=epsc[0:p, :], scale=1.0))
                    k.op(DVE, [SM], [SM], lambda e: e.reciprocal(out=sm[0:p, 5:6], in_=sm[0:p, 5:6]))
                    k.op(DVE, [SM], [SM], lambda e: e.scalar_tensor_tensor(out=sm[0:p, 6:7], in0=sm[0:p, 2:3], scalar=-1.0, in1=sm[0:p, 5:6], op0=ALU.mult, op1=ALU.mult))
                    if _step <= 3:
                        k.end(); return
                    k.op(ACT, [XS, SM], [HB], lambda e: e.activation(out=hb[0:p, :], in_=xs[0:p, :], func=AF.Identity, bias=sm[0:p, 6:7], scale=sm[0:p, 5:6]))
                    k.op(DVE, [HB, GB], [HB], lambda e: e.tensor_tensor(out=hb[0:p, :], in0=hb[0:p, :], in1=gbc[0:p, :], op=ALU.mult))
                    k.op(DVE, [HB, BB], [HB], lambda e: e.tensor_tensor(out=hb[0:p, :], in0=hb[0:p, :], in1=bbc[0:p, :], op=ALU.add))
                    if _step <= 4:
                        k.end(); return
                    for b4 in range(4):
                        def tr(e, b4=b4):
                            ins = None
                            for q in range(4):
                                kc = b4 * 4 + q
                                ins = e.transpose(ps[b4][:, q * 128:q * 128 + p], hb[0:p, kc * 128:(kc + 1) * 128], ident[0:p, 0:p])
                            return ins
                        k.op(PE, [HB] + CR, [PT[b4]], tr)
                        for q in range(4):
                            kc = b4 * 4 + q
                            src = ps[b4][:, q * 128:q * 128 + p]
                            k.op(ACT, [PT[b4]], [STF], lambda e, src=src, kc=kc: e.activation(out=stf[:, kc, j * 128:j * 128 + p], in_=src, func=AF.Identity))
                            k.op(DVE, [STF], [STB], lambda e, kc=kc: e.tensor_copy(out=stb[:, kc, j * 128:j * 128 + p], in_=stf[:, kc, j * 128:j * 128 + p]))
                    if _step <= 5:
                        k.end(); return
                if _step <= 6:
                    k.end(); return
                k.dma(SP, fm(HT32[:, t0:t0 + n]), stf[:, :, 0:n], [STF], [], STF.d)
                k.dma(SP, fm(HTb[:, t0:t0 + n]), stb[:, :, 0:n], [STB], [], STB.d)
            k.end()

        def rope_unit(P, n, xg, XG, ri, tab, TAB, t1, T1, t2, T2, out_tb, out_ap, pr, rr=None, RR=None):
            mm(PT[pr], ps[pr][0:P, 0:n], [(rmat[0:P, ri, 0:P], xg[0:P, 0:n])], [XG])
            k.op(POOL, [XG, TAB], [T1], lambda e: e.tensor_tensor(out=t1[0:P, 0:n], in0=xg[0:P, 0:n], in1=tab[0:P, 0, 0:n], op=ALU.mult))
            k.op(DVE, [PT[pr], TAB], [T2], lambda e: e.tensor_tensor(out=t2[0:P, 0:n], in0=ps[pr][0:P, 0:n], in1=tab[0:P, 1, 0:n], op=ALU.mult))
            if rr is None:
                k.op(POOL, [T1, T2], [out_tb], lambda e: e.tensor_tensor(out=out_ap, in0=t1[0:P, 0:n], in1=t2[0:P, 0:n], op=ALU.add))
            else:
                k.op(POOL, [T1, T2], [T1], lambda e: e.tensor_tensor(out=t1[0:P, 0:n], in0=t1[0:P, 0:n], in1=t2[0:P, 0:n], op=ALU.add))
                k.op(DVE, [T1, RR], [out_tb], lambda e: e.tensor_tensor(out=out_ap, in0=t1[0:P, 0:n], in1=rr[0:P, 0:n], op=ALU.mult))

        def phaseA1(l):
            k.begin()
            co = l * NCOL
            wA, WA = k.sb("wA", [128, 16, 832], BF16, dma="pool")
            wq, _ = k.sb("wq", [128, 4, 1536], BF16)
            wkv, _ = k.sb("wkv", [128, 2, 2048], BF16)
            k.dma(POOL, wA[:], fm(w_in[l, :, 0:832]), [], [WA], WA.d)
            k.dma(POOL, wq[:], fm(w_qb[l]), [], [WA], WA.d)
            k.dma(POOL, wkv[:], fm(w_kvb[l]), [], [WA], WA.d)
            hts = [k.sb(f"hT{i}", [128, 16, 512], BF16, dma=True) for i in range(2)]
            tas = [k.sb(f"ta{i}", [128, 2, 512], F32, dma=True) for i in range(2)]
            cqg, CQG = k.sb("cqg", [128, 4, 512], BF16)
            ckg, CKG = k.sb("ckg", [128, 2, 512], BF16)
            sqk, SQK = k.sb("sqk", [128, 2, 512], BF16)
            sqb = [k.sb(f"sqb{i}", [128, 512], BF16) for i in range(2)]
            xr = [k.sb(f"xr{i}", [128, 512], BF16) for i in range(2)]
            rq, RQ = k.sb("rq", [128, 512], F32)
            rkv, RKV = k.sb("rkv", [128, 512], F32)
            rvc, RVC = k.sb("rvc", [128, 1], F32)
            t1s = [k.sb(f"t1{i}", [128, 512], F32) for i in range(2)]
            t2s = [k.sb(f"t2{i}", [128, 512], F32) for i in range(2)]
            qa, QA = k.sb("qa_st", [128, 8, 512], BF16, dma=True)
            qr, QR = k.sb("qr_st", [128, 4, 512], BF16, dma=True)
            ka, KA = k.sb("ka_st", [128, 8, 512], BF16, dma=True)
            kr, KR = k.sb("kr_st", [64, 512], BF16, dma=True)
            va, VA = k.sb("va_st", [128, 4, 1024], BF16, dma=True)
            ctr = [0]

            def nx():
                ctr[0] += 1
                return ctr[0]
            for bi, (t0, n) in enumerate(feature_blocks()):
                hT, HTB_ = hts[bi % 2]
                ta, TA = tas[bi % 2]
                k.dma(SP, hT[:, :, 0:n], fm(HTb[:, t0:t0 + n]), [], [HTB_], HTB_.d)
                k.dma(SP, ta[:, :, 0:n], tabA[:, :, t0:t0 + n].rearrange("c p t -> p c t"), [], [TA], TA.d)
                for c in range(4):
                    u = nx() % 2
                    mm(PT[u], ps[u][:, 0:n], [(wA[:, kc, c * 128:(c + 1) * 128], hT[:, kc, 0:n]) for kc in range(16)], [WA, HTB_])
                    sb_, SB_ = sqb[c % 2]
                    k.op(ACT, [PT[u]], [SB_], lambda e, u=u, sb_=sb_: e.activation(out=sb_[:, 0:n], in_=ps[u][:, 0:n], func=AF.Square))
                    k.op(ACT, [PT[u]] + CR, [CQG], lambda e, u=u, c=c: e.activation(out=cqg[:, c, 0:n], in_=ps[u][:, 0:n], func=AF.Identity, scale=cols[:, co + C_GQ + c:co + C_GQ + c + 1]))
                    mm(PT[2], ps[2][:, 0:n], [(ones_b[:, :], sb_[:, 0:n])], [SB_], start=(c == 0), stop=(c == 3))
                rsqrt_from(PT[2], ps[2][:, 0:n], RQ, rq[:, 0:n], 1.0 / 512, epsc[:, :])
                for c in range(2):
                    u = nx() % 2
                    mm(PT[u], ps[u][:, 0:n], [(wA[:, kc, 512 + c * 128:512 + (c + 1) * 128], hT[:, kc, 0:n]) for kc in range(16)], [WA, HTB_])
                    k.op(ACT, [PT[u]], [SQK], lambda e, u=u, c=c: e.activation(out=sqk[:, c, 0:n], in_=ps[u][:, 0:n], func=AF.Square))
                    k.op(ACT, [PT[u]] + CR, [CKG], lambda e, u=u, c=c: e.activation(out=ckg[:, c, 0:n], in_=ps[u][:, 0:n], func=AF.Identity, scale=cols[:, co + C_GKV + c:co + C_GKV + c + 1]))
                    mm(PT[3], ps[3][:, 0:n], [(ones_b[:, :], sqk[:, c, 0:n])], [SQK], start=(c == 0), stop=(c == 1))
                rsqrt_from(PT[3], ps[3][:, 0:n], RKV, rkv[:, 0:n], 1.0 / 256, epsc[:, :])
                for h in range(8):
                    u = nx() % 2
                    mm(PT[u], ps[u][:, 0:n], [(wq[:, kc, h * 128:(h + 1) * 128], cqg[:, kc, 0:n]) for kc in range(4)], [WA, CQG])
                    k.op(DVE, [PT[u], RQ], [QA], lambda e, u=u, h=h: e.tensor_tensor(out=qa[:, h, 0:n], in0=ps[u][:, 0:n], in1=rq[:, 0:n], op=ALU.mult))
                for j in range(4):
                    u = nx() % 2
                    mm(PT[u], ps[u][:, 0:n], [(wq[:, kc, 1024 + j * 128:1024 + (j + 1) * 128], cqg[:, kc, 0:n]) for kc in range(4)], [WA, CQG])
                    xr_, XR_ = xr[j % 2]
                    k.op(DVE, [PT[u], RQ], [XR_], lambda e, u=u, xr_=xr_: e.tensor_tensor(out=xr_[:, 0:n], in0=ps[u][:, 0:n], in1=rq[:, 0:n], op=ALU.mult))
                    t1, T1 = t1s[j % 2]
                    t2, T2 = t2s[j % 2]
                    rope_unit(128, n, xr_, XR_, 0, ta, TA, t1, T1, t2, T2, QR, qr[:, j, 0:n], 4 + j % 2)
                for h in range(8):
                    u = nx() % 2
                    mm(PT[u], ps[u][:, 0:n], [(wkv[:, kc, h * 128:(h + 1) * 128], ckg[:, kc, 0:n]) for kc in range(2)], [WA, CKG])
                    k.op(DVE, [PT[u], RKV], [KA], lambda e, u=u, h=h: e.tensor_tensor(out=ka[:, h, 0:n], in0=ps[u][:, 0:n], in1=rkv[:, 0:n], op=ALU.mult))
                u = nx() % 2
                mm(PT[u], ps[u][0:64, 0:n], [(wA[:, kc, 768:832], hT[:, kc, 0:n]) for kc in range(16)], [WA, HTB_])
                xr_, XR_ = xr[0]
                k.op(ACT, [PT[u]], [XR_], lambda e, u=u, xr_=xr_: e.activation(out=xr_[0:64, 0:n], in_=ps[u][0:64, 0:n], func=AF.Identity))
                rope_unit(64, n, xr_, XR_, 0, ta, TA, t1s[0][0], t1s[0][1], t2s[0][0], t2s[0][1], KR, kr[0:64, 0:n], 4)
                ntile = (n + 127) // 128
                for j in range(ntile):
                    p = min(128, n - j * 128)
                    mm(PT[6], ps[6][0:p, 0:1], [(sqk[:, c, j * 128:j * 128 + p], ones_b[:, 0:1]) for c in range(2)], [SQK])
                    k.op(ACT, [PT[6]] + CR, [RVC], lambda e: e.activation(out=rvc[0:p, :], in_=ps[6][0:p, 0:1], func=AF.Sqrt, bias=epsc[0:p, :], scale=1.0 / 256))
                    k.op(DVE, [RVC], [RVC], lambda e: e.reciprocal(out=rvc[0:p, :], in_=rvc[0:p, :]))
                    for hf in range(2):
                        u = nx() % 2
                        mm(PT[u], ps[u][0:p, :], [(ckg[:, c, j * 128:j * 128 + p], wkv[:, c, 1024 + hf * 512:1024 + (hf + 1) * 512]) for c in range(2)], [WA, CKG])
                        k.op(ACT, [PT[u], RVC], [VA], lambda e, u=u, hf=hf: e.activation(out=va[0:p, j, hf * 512:(hf + 1) * 512], in_=ps[u][0:p, :], func=AF.Identity, scale=rvc[0:p, 0:1]))
                k.dma(SP, QaT[:, :, t0:t0 + n].rearrange("h p t -> p h t"), qa[:, :, 0:n], [QA], [], QA.d)
                k.dma(SP, QrT[:, :, t0:t0 + n].rearrange("h p t -> p h t"), qr[:, :, 0:n], [QR], [], QR.d)
                k.dma(SP, KaT[:, :, t0:t0 + n].rearrange("h p t -> p h t"), ka[:, :, 0:n], [KA], [], KA.d)
                k.dma(SP, KrT[:, t0:t0 + n], kr[0:64, 0:n], [KR], [], KR.d)
                if n % 128 == 0:
                    k.dma(SP, Va[t0:t0 + n, :].rearrange("(j p) f -> p j f", p=128), va[:, 0:n // 128, :], [VA], [], VA.d)
                else:
                    k.dma(SP, Va[t0:t0 + n, :], va[0:n, 0, :], [VA], [], VA.d)
            k.end()

        def phaseA2(l):
            k.begin()
            co = l * NCOL
            wB, WB = k.sb("wB", [128, 16, 1536], BF16, dma="pool")
            k.dma(POOL, wB[:], fm(w_in[l, :, 832:2368]), [], [WB], WB.d)
            hts = [k.sb(f"hT{i}", [128, 16, 512], BF16, dma=True) for i in range(2)]
            tbs = [k.sb(f"tb{i}", [128, 2, 512], F32, dma=True) for i in range(2)]
            sqb = [k.sb(f"sqb{i}", [128, 512], BF16) for i in range(2)]
            xg = [k.sb(f"xg{i}", [128, 512], BF16) for i in range(2)]
            rrs = [k.sb(f"rr{i}", [128, 512], F32) for i in range(2)]
            t1s = [k.sb(f"t1{i}", [128, 512], F32) for i in range(2)]
            t2s = [k.sb(f"t2{i}", [128, 512], F32) for i in range(2)]
            gq, GQ = k.sb("gq_st", [128, 10, 512], BF16, dma=True)
            gv, GVS = k.sb("gv_st", [128, 4, 256], BF16, dma=True)
            for bi, (t0, n) in enumerate(feature_blocks()):
                hT, HTB_ = hts[bi % 2]
                tb, TBL = tbs[bi % 2]
                k.dma(SP, hT[:, :, 0:n], fm(HTb[:, t0:t0 + n]), [], [HTB_], HTB_.d)
                k.dma(SP, tb[:, :, 0:n], tabB[:, :, t0:t0 + n].rearrange("c p t -> p c t"), [], [TBL], TBL.d)
                for h in range(10):
                    u = h % 2
                    gcol = co + (C_GQQ if h < 8 else C_GQK)
                    mm(PT[u], ps[u][:, 0:n], [(wB[:, kc, h * 128:(h + 1) * 128], hT[:, kc, 0:n]) for kc in range(16)], [WB, HTB_])
                    sb_, SB_ = sqb[u]
                    xg_, XG_ = xg[u]
                    rr, RR = rrs[u]
                    k.op(ACT, [PT[u]], [SB_], lambda e, u=u, sb_=sb_: e.activation(out=sb_[:, 0:n], in_=ps[u][:, 0:n], func=AF.Square))
                    k.op(ACT, [PT[u]] + CR, [XG_], lambda e, u=u, xg_=xg_, gcol=gcol: e.activation(out=xg_[:, 0:n], in_=ps[u][:, 0:n], func=AF.Identity, scale=cols[:, gcol:gcol + 1]))
                    mm(PT[2 + u], ps[2 + u][:, 0:n], [(ones_b[:, :], sb_[:, 0:n])], [SB_])
                    rsqrt_from(PT[2 + u], ps[2 + u][:, 0:n], RR, rr[:, 0:n], 1.0 / 128, epsc[:, :])
                    rope_unit(128, n, xg_, XG_, 1, tb, TBL, t1s[u][0], t1s[u][1], t2s[u][0], t2s[u][1], GQ, gq[:, h, 0:n], 4 + u, rr, RR)
                ntile = (n + 127) // 128
                for j in range(ntile):
                    p = min(128, n - j * 128)
                    u = 6 + j % 2
                    mm(PT[u], ps[u][0:p, 0:256], [(hT[:, kc, j * 128:j * 128 + p], wB[:, kc, 1280:1536]) for kc in range(16)], [WB, HTB_])
                    k.op(ACT, [PT[u]], [GVS], lambda e, u=u: e.activation(out=gv[0:p, j, :], in_=ps[u][0:p, 0:256], func=AF.Identity))
                k.dma(SP, GQT[:, :, t0:t0 + n].rearrange("h p t -> p h t"), gq[:, 0:8, 0:n], [GQ], [], GQ.d)
                k.dma(SP, GKT[:, :, t0:t0 + n].rearrange("h p t -> p h t"), gq[:, 8:10, 0:n], [GQ], [], GQ.d)
                if n % 128 == 0:
                    k.dma(SP, GV[t0:t0 + n, :].rearrange("(j p) f -> p j f", p=128), gv[:, 0:n // 128, :], [GVS], [], GVS.d)
                else:
                    k.dma(SP, GV[t0:t0 + n, :], gv[0:n, 0, :], [GVS], [], GVS.d)
            k.end()

        def phaseB1(l):
            k.begin()
            kaS, KV = k.sb("kaS", [128, 8, T], BF16, dma=True)
            kr2, _ = k.sb("kr2", [128, T], BF16)
            gkS, _ = k.sb("gkS", [128, 2, T], BF16)
            vaS, _ = k.sb("vaS", [128, 17, 1024], BF16)
            gvS, _ = k.sb("gvS", [128, 17, 256], BF16)
            qsl = []
            for i in range(2):
                a_, A_ = k.sb(f"qa{i}", [128, 8, 512], BF16, dma=True)
                r_, _ = k.sb(f"qr{i}", [128, 4, 512], BF16)
                g_, _ = k.sb(f"gq{i}", [128, 8, 512], BF16)
                qsl.append((a_, r_, g_, A_))
            pts = [k.sb(f"pt{i}", [128, 512], BF16) for i in range(3)]
            rl, RL = k.sb("rl", [128, 512], F32)
            ots = [k.sb(f"ot{i}", [128, 512], F32, dma=True) for i in range(4)]
            qi = 0
            oi = 0
            hcount = 0
            for s in range(SPC):
                r0, m0 = s * SEQ, NR + s * NMETA
                for (dst, src) in [
                    (kaS[:, :, 0:SEQ], KaT[:, :, r0:r0 + SEQ].rearrange("h p t -> p h t")),
                    (kaS[:, :, SEQ:T], KaT[:, :, m0:m0 + NMETA].rearrange("h p t -> p h t")),
                    (kr2[0:64, 0:SEQ], KrT[:, r0:r0 + SEQ]), (kr2[64:128, 0:SEQ], KrT[:, r0:r0 + SEQ]),
                    (kr2[0:64, SEQ:T], KrT[:, m0:m0 + NMETA]), (kr2[64:128, SEQ:T], KrT[:, m0:m0 + NMETA]),
                    (gkS[:, :, 0:SEQ], GKT[:, :, r0:r0 + SEQ].rearrange("h p t -> p h t")),
                    (gkS[:, :, SEQ:T], GKT[:, :, m0:m0 + NMETA].rearrange("h p t -> p h t")),
                    (vaS[:, 0:16, :], Va[r0:r0 + SEQ, :].rearrange("(j p) f -> p j f", p=128)),
                    (vaS[0:16, 16, :], Va[m0:m0 + NMETA, :]),
                    (gvS[:, 0:16, :], GV[r0:r0 + SEQ, :].rearrange("(j p) f -> p j f", p=128)),
                    (gvS[0:16, 16, :], GV[m0:m0 + NMETA, :]),
                ]:
                    k.dma(SP, dst, src, [], [KV], KV.d)
                qblocks = [(r0 + i * 512, 512) for i in range(4)] + [(m0, NMETA)]
                for (t0, nq) in qblocks:
                    qa_, qr_, gq_, QS = qsl[qi % 2]
                    qi += 1
                    k.dma(SP, qa_[:, :, 0:nq], QaT[:, :, t0:t0 + nq].rearrange("h p t -> p h t"), [], [QS], QS.d)
                    k.dma(SP, qr_[:, :, 0:nq], QrT[:, :, t0:t0 + nq].rearrange("h p t -> p h t"), [], [QS], QS.d)
                    k.dma(SP, gq_[:, :, 0:nq], GQT[:, :, t0:t0 + nq].rearrange("h p t -> p h t"), [], [QS], QS.d)
                    for hh in range(16):
                        o = hcount % 2
                        hcount += 1
                        PO, PL = PT[3 + o], PT[5 + o]
                        pso, psl = ps[3 + o], ps[5 + o]

                        def emitS(kt, hh=hh):
                            k0 = kt * 128
                            kp = 128 if kt < 16 else NMETA
                            i = kt % 3
                            if hh < 8:
                                pb = 64 * (hh % 2)
                                pairs = [(kaS[:, hh, k0:k0 + kp], qa_[:, hh, 0:nq]),
                                         (kr2[pb:pb + 64, k0:k0 + kp], qr_[pb:pb + 64, hh // 2, 0:nq])]
                            else:
                                g = hh - 8
                                pairs = [(gkS[:, g // 4, k0:k0 + kp], gq_[:, g, 0:nq])]
                            mm(PT[i], ps[i][0:kp, 0:nq], pairs, [KV, QS])

                        def emitE(kt, hh=hh):
                            kp = 128 if kt < 16 else NMETA
                            i = kt % 3
                            pt_, PT_ = pts[i]
                            sc = MLA_SCALE if hh < 8 else GQA_SCALE
                            k.op(ACT, [PT[i]], [PT_], lambda e: e.activation(out=pt_[0:kp, 0:nq], in_=ps[i][0:kp, 0:nq], func=AF.Exp, scale=sc))

                        def emitO(kt, hh=hh):
                            kp = 128 if kt < 16 else NMETA
                            i = kt % 3
                            pt_, PT_ = pts[i]
                            if hh < 8:
                                vv = vaS[0:kp, kt, hh * 128:(hh + 1) * 128]
                            else:
                                g = (hh - 8) // 4
                                vv = gvS[0:kp, kt, g * 128:(g + 1) * 128]
                            mm(PO, pso[:, 0:nq], [(vv, pt_[0:kp, 0:nq])], [KV, PT_], start=(kt == 0), stop=(kt == 16))
                            mm(PL, psl[:, 0:nq], [(ones_b[0:kp, :], pt_[0:kp, 0:nq])], [PT_], start=(kt == 0), stop=(kt == 16))
                        emitS(0)
                        emitS(1)
                        for kt in range(17):
                            emitE(kt)
                            if kt + 2 < 17:
                                emitS(kt + 2)
                            emitO(kt)
                        ot_, OT_ = ots[oi % 4]
                        oi += 1
                        k.op(DVE, [PL], [RL], lambda e: e.reciprocal(out=rl[:, 0:nq], in_=psl[:, 0:nq]))
                        k.op(DVE, [PO, RL], [OT_], lambda e, ot_=ot_: e.tensor_tensor(out=ot_[:, 0:nq], in0=pso[:, 0:nq], in1=rl[:, 0:nq], op=ALU.mult))
                        k.dma(SP, OT[hh * 128:(hh + 1) * 128, t0:t0 + nq], ot_[:, 0:nq], [OT_], [], OT_.d)
            k.end()

        def ln_fm(n, z, Z, zb, ZB, gc, bc, eps_ap, tmps):
            (zsq, mean, MEAN, rstd, RSTD, nmr, NMR) = tmps
            mm(PT[6], ps[6][:, 0:n], [(ones_f[:, :], z(kc)) for kc in range(16)], [Z])
            for kc in range(16):
                zs, ZS = zsq[kc % 2]
                k.op(ACT, [Z], [ZS], lambda e, zs=zs, kc=kc: e.activation(out=zs[:, 0:n], in_=z(kc), func=AF.Square))
                mm(PT[7], ps[7][:, 0:n], [(ones_f[:, :], zs[:, 0:n])], [ZS], start=(kc == 0), stop=(kc == 15))
            k.op(ACT, [PT[6]], [MEAN], lambda e: e.activation(out=mean[:, 0:n], in_=ps[6][:, 0:n], func=AF.Identity, scale=1.0 / D))
            k.op(DVE, [MEAN], [NMR], lambda e: e.tensor_tensor(out=nmr[:, 0:n], in0=mean[:, 0:n], in1=mean[:, 0:n], op=ALU.mult))
            k.op(DVE, [PT[7], NMR], [RSTD], lambda e: e.scalar_tensor_tensor(out=rstd[:, 0:n], in0=ps[7][:, 0:n], scalar=1.0 / D, in1=nmr[:, 0:n], op0=ALU.mult, op1=ALU.subtract))
            k.op(ACT, [RSTD] + CR, [RSTD], lambda e: e.activation(out=rstd[:, 0:n], in_=rstd[:, 0:n], func=AF.Sqrt, bias=eps_ap, scale=1.0))
            k.op(DVE, [RSTD], [RSTD], lambda e: e.reciprocal(out=rstd[:, 0:n], in_=rstd[:, 0:n]))
            k.op(DVE, [MEAN, RSTD], [NMR], lambda e: e.tensor_tensor(out=nmr[:, 0:n], in0=mean[:, 0:n], in1=rstd[:, 0:n], op=ALU.mult))
            for kc in range(16):
                k.op(DVE, [Z, RSTD], [Z], lambda e, kc=kc: e.tensor_tensor(out=z(kc), in0=z(kc), in1=rstd[:, 0:n], op=ALU.mult))
                k.op(POOL, [Z, NMR], [Z], lambda e, kc=kc: e.tensor_tensor(out=z(kc), in0=z(kc), in1=nmr[:, 0:n], op=ALU.subtract))
                k.op(ACT, [Z] + CR, [Z], lambda e, kc=kc: e.activation(out=z(kc), in_=z(kc), func=AF.Identity, bias=cols[:, bc + kc:bc + kc + 1], scale=cols[:, gc + kc:gc + kc + 1]))
                if zb is not None:
                    k.op(POOL, [Z], [ZB], lambda e, kc=kc: e.tensor_copy(out=zb(kc), in_=z(kc)))

        def ln_tmps():
            zsq = [k.sb(f"zsq{i}", [128, 512], F32) for i in range(2)]
            mean, MEAN = k.sb("mean", [128, 512], F32)
            rstd, RSTD = k.sb("rstd", [128, 512], F32)
            nmr, NMR = k.sb("nmr", [128, 512], F32)
            return (zsq, mean, MEAN, rstd, RSTD, nmr, NMR)

        def phaseB2(l):
            k.begin()
            co = l * NCOL
            wo, WO = k.sb("wo", [128, 16, D], BF16, dma="pool")
            k.dma(POOL, wo[:], fm(w_out[l]), [], [WO], WO.d)
            ot, OTB = k.sb("ot", [128, 16, 512], F32, dma=True)
            onb, ONB = k.sb("onb", [128, 16, 512], BF16)
            hz, HZ = k.sb("hz", [128, 16, 512], F32, dma=True)
            hzb, HZB = k.sb("hzb", [128, 16, 512], BF16, dma=True)
            sqb = [k.sb(f"sqb{i}", [128, 512], BF16) for i in range(2)]
            ra, RA = k.sb("ra", [128, 512], F32)
            rb, RB = k.sb("rb", [128, 512], F32)
            tmps = ln_tmps()
            for (t0, n) in feature_blocks():
                k.dma(SP, ot[:, :, 0:n], fm(OT[:, t0:t0 + n]), [], [OTB], OTB.d)
                k.dma(SP, hz[:, :, 0:n], fm(HT32[:, t0:t0 + n]), [], [HZ], HZ.d)
                for grp in range(2):
                    for c in range(8):
                        kc = grp * 8 + c
                        sb_, SB_ = sqb[kc % 2]
                        k.op(ACT, [OTB], [SB_], lambda e, sb_=sb_, kc=kc: e.activation(out=sb_[:, 0:n], in_=ot[:, kc, 0:n], func=AF.Square))
                        mm(PT[2 + grp], ps[2 + grp][:, 0:n], [(ones_b[:, :], sb_[:, 0:n])], [SB_], start=(c == 0), stop=(c == 7))
                rsqrt_from(PT[2], ps[2][:, 0:n], RA, ra[:, 0:n], 1.0 / 1024, epsc[:, :])
                rsqrt_from(PT[3], ps[3][:, 0:n], RB, rb[:, 0:n], 1.0 / 1024, epsc[:, :])
                for kc in range(16):
                    r_, R_ = (ra, RA) if kc < 8 else (rb, RB)
                    gcx = co + C_GOA + kc
                    k.op(DVE, [OTB, R_] + CR, [ONB], lambda e, kc=kc, r_=r_, gcx=gcx: e.scalar_tensor_tensor(out=onb[:, kc, 0:n], in0=ot[:, kc, 0:n], scalar=cols[:, gcx:gcx + 1], in1=r_[:, 0:n], op0=ALU.mult, op1=ALU.mult))
                for fc in range(16):
                    u = fc % 2
                    mm(PT[u], ps[u][:, 0:n], [(wo[:, kc, fc * 128:(fc + 1) * 128], onb[:, kc, 0:n]) for kc in range(16)], [WO, ONB])
                    k.op(DVE, [PT[u], HZ], [HZ], lambda e, u=u, fc=fc: e.scalar_tensor_tensor(out=hz[:, fc, 0:n], in0=hz[:, fc, 0:n], scalar=ALPHA, in1=ps[u][:, 0:n], op0=ALU.mult, op1=ALU.add))
                ln_fm(n, lambda kc: hz[:, kc, 0:n], HZ, lambda kc: hzb[:, kc, 0:n], HZB, co + C_L1G, co + C_L1B, epsc[:, :], tmps)
                k.dma(SP, fm(HT32[:, t0:t0 + n]), hz[:, :, 0:n], [HZ], [], HZ.d)
                k.dma(SP, fm(HTb[:, t0:t0 + n]), hzb[:, :, 0:n], [HZB], [], HZB.d)
            k.end()

        def phaseC(l, last):
            k.begin()
            co = l * NCOL
            TBM = 1024 + NM
            hT, HT_ = k.sb("h1T", [128, 16, TBM], BF16, dma=True)
            ya, YA = k.sb("yacc", [128, 16, TBM], F32, dma=True)
            he, HE = k.sb("heT", [128, 8, TBM], BF16)
            cbs = [k.sb(f"cbc{i}", [128, TBM], BF16) for i in range(2)]
            wg = [k.sb(f"wg{i}", [128, 16, 256], BF16, dma="pool") for i in range(2)]
            wu = [k.sb(f"wu{i}", [128, 16, 256], BF16, dma="pool") for i in range(2)]
            wd = [k.sb(f"wd{i}", [128, 8, 512], BF16, dma="pool") for i in range(2)]
            sgs = [k.sb(f"sg{i}", [128, 512], BF16) for i in range(2)]
            tts = [k.sb(f"tt{i}", [128, 512], BF16) for i in range(2)]
            cmbT, CMBT = k.sb("cmbT", [NE, TBM], F32)
            rt, RT = k.sb("rt", [128, 12, NE], F32)
            rs, RS = k.sb("rs", [128, 8], F32)
            tmps = ln_tmps()
            if last:
                osts = [k.sb(f"ost{i}", [128, 1024], F32, dma=True) for i in range(1)]
            nblk = NR // 1024
            wcnt = [0, 0]
            for b in range(nblk):
                subs = [(b * 1024, 512, 0), (b * 1024 + 512, 512, 512)]
                if b == nblk - 1:
                    subs.append((NR, NM, 1024))
                for (t0, n, c0) in subs:
                    k.dma(SP, hT[:, :, c0:c0 + n], fm(HTb[:, t0:t0 + n]), [], [HT_], HT_.d)
                    k.dma(SP, ya[:, :, c0:c0 + n], fm(HT32[:, t0:t0 + n]), [], [YA], YA.d)
                for (t0, n, c0) in subs:
                    for j in range((n + 127) // 128):
                        p = min(128, n - j * 128)
                        cc = c0 + j * 128
                        mm(PT[0], ps[0][0:p, 0:NE], [(ya[:, kc, cc:cc + p], wrt[:, kc, :]) for kc in range(16)], [YA])
                        S_, SEL_, M1, EQ, S2, M2, MSK, IS1, IS2 = [rt[0:p, i, :] for i in range(9)]
                        k.op(ACT, [PT[0]], [RT], lambda e: e.activation(out=S_, in_=ps[0][0:p, 0:NE], func=AF.Sigmoid))
                        k.op(DVE, [RT] + CR, [RT], lambda e: e.tensor_tensor(out=SEL_, in0=S_, in1=rb_bc[0:p, :], op=ALU.add))
                        sel3 = SEL_.rearrange("p (g e) -> p g e", e=4)
                        k.op(DVE, [RT], [RS], lambda e: e.tensor_reduce(out=rs[0:p, 0:4], in_=sel3, axis=AX.X, op=ALU.max))
                        k.op(DVE, [RT, RS], [RT], lambda e: e.tensor_tensor(out=EQ.rearrange("p (g e) -> p g e", e=4), in0=sel3, in1=rs[0:p, 0:4].unsqueeze(2).to_broadcast([p, 4, 4]), op=ALU.is_equal))
                        k.op(DVE, [RT], [RT], lambda e: e.scalar_tensor_tensor(out=S2, in0=EQ, scalar=NEG, in1=SEL_, op0=ALU.mult, op1=ALU.add))
                        k.op(DVE, [RT], [RS], lambda e: e.tensor_reduce(out=rs[0:p, 4:8], in_=S2.rearrange("p (g e) -> p g e", e=4), axis=AX.X, op=ALU.max))
                        k.op(DVE, [RS], [RS], lambda e: e.tensor_tensor(out=rs[0:p, 0:4], in0=rs[0:p, 0:4], in1=rs[0:p, 4:8], op=ALU.add))
                        k.op(DVE, [RS], [RS], lambda e: e.tensor_reduce(out=rs[0:p, 4:5], in_=rs[0:p, 0:4], axis=AX.X, op=ALU.max))
                        k.op(DVE, [RS], [RS], lambda e: e.tensor_scalar(out=rs[0:p, 0:4], in0=rs[0:p, 0:4], scalar1=rs[0:p, 4:5], scalar2=None, op0=ALU.is_equal))
                        k.op(DVE, [RS], [RS], lambda e: e.tensor_scalar(out=rs[0:p, 0:4], in0=rs[0:p, 0:4], scalar1=-1.0, scalar2=-NEG, op0=ALU.add, op1=ALU.mult))
                        k.op(DVE, [RT, RS], [RT], lambda e: e.tensor_tensor(out=MSK.rearrange("p (g e) -> p g e", e=4), in0=sel3, in1=rs[0:p, 0:4].unsqueeze(2).to_broadcast([p, 4, 4]), op=ALU.add))
                        k.op(DVE, [RT], [RS], lambda e: e.tensor_reduce(out=rs[0:p, 5:6], in_=MSK, axis=AX.X, op=ALU.max))
                        k.op(DVE, [RT, RS], [RT], lambda e: e.tensor_scalar(out=IS1, in0=MSK, scalar1=rs[0:p, 5:6], scalar2=None, op0=ALU.is_equal))
                        k.op(DVE, [RT], [RT], lambda e: e.scalar_tensor_tensor(out=MSK, in0=IS1, scalar=NEG, in1=MSK, op0=ALU.mult, op1=ALU.add))
                        k.op(DVE, [RT], [RS], lambda e: e.tensor_reduce(out=rs[0:p, 6:7], in_=MSK, axis=AX.X, op=ALU.max))
                        k.op(DVE, [RT, RS], [RT], lambda e: e.tensor_scalar(out=IS2, in0=MSK, scalar1=rs[0:p, 6:7], scalar2=None, op0=ALU.is_equal))
                        k.op(DVE, [RT], [RT], lambda e: e.tensor_tensor(out=IS1, in0=IS1, in1=IS2, op=ALU.add))
                        k.op(DVE, [RT], [RT], lambda e: e.tensor_tensor(out=IS1, in0=IS1, in1=S_, op=ALU.mult))
                        k.op(DVE, [RT], [RS], lambda e: e.tensor_reduce(out=rs[0:p, 7:8], in_=IS1, axis=AX.X, op=ALU.add))
                        k.op(DVE, [RS], [RS], lambda e: e.reciprocal(out=rs[0:p, 7:8], in_=rs[0:p, 7:8]))
                        k.op(DVE, [RT, RS], [RT], lambda e: e.tensor_scalar(out=IS2, in0=IS1, scalar1=rs[0:p, 7:8], scalar2=1.0 / ALPHA, op0=ALU.mult, op1=ALU.mult))
                        k.op(PE, [RT] + CR, [PT[1]], lambda e: e.transpose(ps[1][0:NE, 0:p], IS2, ident[0:p, 0:p]))
                        k.op(ACT, [PT[1]], [CMBT], lambda e: e.activation(out=cmbT[:, cc:cc + p], in_=ps[1][0:NE, 0:p], func=AF.Identity))
                def load_gu(i):
                    if i >= NE * 4:
                        return
                    ex_, q_ = divmod(i, 4)
                    g_, G_ = wg[i % 2]
                    u_, U_ = wu[i % 2]
                    k.dma(POOL, g_[:], fm(w_gate[l, ex_, :, q_ * 256:(q_ + 1) * 256]), [], [G_], G_.d)
                    k.dma(POOL, u_[:], fm(w_up[l, ex_, :, q_ * 256:(q_ + 1) * 256]), [], [U_], U_.d)

                def load_d(i):
                    if i >= NE * 4:
                        return
                    ex_, q_ = divmod(i, 4)
                    d_, D_ = wd[i % 2]
                    k.dma(POOL, d_[:], w_down[l, ex_, :, q_ * 512:(q_ + 1) * 512].rearrange("(fc p) f -> p fc f", p=128), [], [D_], D_.d)
                for ex in range(NE):
                    cb, CB = cbs[ex % 2]
                    for si, (t0, n, c0) in enumerate(subs):
                        u = 6 + si % 2
                        mm(PT[u], ps[u][:, 0:n], [(sel[:, ex * 128:(ex + 1) * 128], cmbT[:, c0:c0 + n])], [CMBT])
                        k.op(ACT, [PT[u]], [CB], lambda e, u=u, c0=c0, n=n, cb=cb: e.activation(out=cb[:, c0:c0 + n], in_=ps[u][:, 0:n], func=AF.Identity))
                    for q4 in range(4):
                        if ex == 0 and q4 == 0:
                            load_gu(0)
                        load_gu(ex * 4 + q4 + 1)
                        g_, G_ = wg[(ex * 4 + q4) % 2]
                        u_, U_ = wu[(ex * 4 + q4) % 2]
                        for f2 in range(2):
                            ffc = q4 * 2 + f2
                            for si, (t0, n, c0) in enumerate(subs):
                                pg, pu = si % 2, 2 + si % 2
                                mm(PT[pg], ps[pg][:, 0:n], [(g_[:, kc, f2 * 128:(f2 + 1) * 128], hT[:, kc, c0:c0 + n]) for kc in range(16)], [G_, HT_])
                                mm(PT[pu], ps[pu][:, 0:n], [(u_[:, kc, f2 * 128:(f2 + 1) * 128], hT[:, kc, c0:c0 + n]) for kc in range(16)], [U_, HT_])
                                sg, SG = sgs[si % 2]
                                tt, TT = tts[si % 2]
                                k.op(ACT, [PT[pg]], [SG], lambda e, pg=pg, sg=sg, n=n: e.activation(out=sg[:, 0:n], in_=ps[pg][:, 0:n], func=AF.Silu))
                                k.op(DVE, [PT[pu], CB], [TT], lambda e, pu=pu, tt=tt, n=n, c0=c0, cb=cb: e.tensor_tensor(out=tt[:, 0:n], in0=ps[pu][:, 0:n], in1=cb[:, c0:c0 + n], op=ALU.mult))
                                k.op(POOL, [SG, TT], [HE], lambda e, sg=sg, tt=tt, n=n, c0=c0, ffc=ffc: e.tensor_tensor(out=he[:, ffc, c0:c0 + n], in0=sg[:, 0:n], in1=tt[:, 0:n], op=ALU.mult))
                    for q4 in range(4):
                        if ex == 0 and q4 == 0:
                            load_d(0)
                        load_d(ex * 4 + q4 + 1)
                        d_, D_ = wd[(ex * 4 + q4) % 2]
                        for f4 in range(4):
                            fc = q4 * 4 + f4
                            for si, (t0, n, c0) in enumerate(subs):
                                pd = 4 + (f4 * 3 + si) % 2
                                mm(PT[pd], ps[pd][:, 0:n], [(d_[:, ffc, f4 * 128:(f4 + 1) * 128], he[:, ffc, c0:c0 + n]) for ffc in range(8)], [D_, HE])
                                k.op(DVE, [PT[pd], YA], [YA], lambda e, pd=pd, fc=fc, n=n, c0=c0: e.tensor_tensor(out=ya[:, fc, c0:c0 + n], in0=ya[:, fc, c0:c0 + n], in1=ps[pd][:, 0:n], op=ALU.add))
                for (t0, n, c0) in subs:
                    ln_fm(n, lambda kc, c0=c0, n=n: ya[:, kc, c0:c0 + n], YA,
                          (None if last else (lambda kc, c0=c0, n=n: hT[:, kc, c0:c0 + n])), HT_,
                          co + C_L2G, co + C_L2B, eps2c[:, :], tmps)
                    if not last:
                        k.dma(SP, fm(HT32[:, t0:t0 + n]), ya[:, :, c0:c0 + n], [YA], [], YA.d)
                        k.dma(SP, fm(HTb[:, t0:t0 + n]), hT[:, :, c0:c0 + n], [HT_], [], HT_.d)
                    elif t0 < NR:
                        for j in range(n // 128):
                            os_, OS_ = osts[0]
                            s, s0 = divmod(t0 + j * 128, SEQ)
                            for b4 in range(4):
                                def tr(e, b4=b4, j=j, c0=c0):
                                    ins = None
                                    for q in range(4):
                                        kc = b4 * 4 + q
                                        ins = e.transpose(ps[b4][:, q * 128:(q + 1) * 128], ya[:, kc, c0 + j * 128:c0 + (j + 1) * 128], ident[:, :])
                                    return ins
                                k.op(PE, [YA] + CR, [PT[b4]], tr)
                                hb4 = b4 % 2
                                if b4 % 2 == 0:
                                    k.op(ACT, [PT[b4]], [OS_], lambda e, b4=b4, os_=os_, hb4=hb4: e.activation(out=os_[:, hb4 * 512:(hb4 + 1) * 512], in_=ps[b4][:, :], func=AF.Identity))
                                else:
                                    k.op(DVE, [PT[b4]], [OS_], lambda e, b4=b4, os_=os_, hb4=hb4: e.tensor_copy(out=os_[:, hb4 * 512:(hb4 + 1) * 512], in_=ps[b4][:, :]))
                                    hf = b4 // 2
                                    k.dma(SP, out[s, s0:s0 + 128, hf * 1024:(hf + 1) * 1024], os_[:, :], [OS_], [], OS_.d)
            k.end()

        stop = dbg if isinstance(dbg, str) else None
        if stop == "const":
            k.barrier()
            return nc
        phase0()
        for l in range(depth):
            if stop == "p0":
                break
            phaseA1(l)
            phaseA2(l)
            if stop == "a":
                break
            phaseB1(l)
            if stop == "b1":
                break
            phaseB2(l)
            if stop == "b2":
                break
            phaseC(l, l == depth - 1)
    return nc


def _rope_tables(SPC):
    NR, NM = SPC * SEQ, SPC * NMETA
    ROWS = SEQ // 64
    pr = np.concatenate([np.tile(np.repeat(np.arange(ROWS, dtype=np.float32), 64), SPC), np.full((NM,), -1.0, np.float32)])
    pc = np.concatenate([np.tile(np.tile(np.arange(64, dtype=np.float32), ROWS), SPC), np.tile(np.arange(NMETA, dtype=np.float32), SPC)])

    def tab(rot):
        n = rot // 4
        inv = (np.float32(10000.0) ** (-np.arange(n, dtype=np.float32) / np.float32(n))).astype(np.float32)
        ang = np.concatenate([pr[:, None] * inv, pc[:, None] * inv], axis=-1).astype(np.float32)
        c, s = np.cos(ang).T.astype(np.float32), np.sin(ang).T.astype(np.float32)
        reps = 128 // (rot // 2)
        return np.ascontiguousarray(np.stack([np.concatenate([c] * reps, 0), np.concatenate([s] * reps, 0)], 0))
    return tab(64), tab(128)


def _rmats():
    def R(half):
        m = np.zeros((2 * half, 2 * half), np.float32)
        for i in range(half):
            m[i + half, i] = -1.0
            m[i, i + half] = 1.0
        return m
    ra = np.zeros((128, 128), np.float32)
    ra[0:64, 0:64] = R(32)
    ra[64:128, 64:128] = R(32)
    return np.stack([ra, R(64)], 0)


def _prep_shared(inp, depth):
    f = lambda a: np.ascontiguousarray(np.asarray(a, dtype=np.float32))
    qperm = np.concatenate([np.arange(h * 192, h * 192 + 128) for h in range(8)] + [np.arange(h * 192 + 128, h * 192 + 192) for h in range(8)])
    kperm = np.concatenate([np.arange(h * 256, h * 256 + 128) for h in range(8)] + [np.arange(h * 256 + 128, h * 256 + 256) for h in range(8)])
    cols = np.zeros((128, depth * NCOL), np.float32)

    def put(l, off, v):
        v = np.asarray(v, np.float32)
        c = v.shape[0] // 128
        cols[:, l * NCOL + off:l * NCOL + off + c] = v.reshape(c, 128).T
    for l in range(depth):
        put(l, C_GQ, inp["g_q_lora"][l]); put(l, C_GKV, inp["g_kv_lora"][l])
        put(l, C_GQQ, inp["g_qk_q"][l]); put(l, C_GQK, inp["g_qk_k"][l])
        put(l, C_GOA, inp["g_out_mla"][l]); put(l, C_GOB, inp["g_out_gqa"][l])
        put(l, C_L1G, inp["ln1_g"][l]); put(l, C_L1B, inp["ln1_b"][l])
        put(l, C_L2G, inp["ln2_g"][l]); put(l, C_L2B, inp["ln2_b"][l])
    sel = np.zeros((NE, NE, 128), np.float32)
    for e in range(NE):
        sel[e, e, :] = 1.0
    return {
        "meta": f(inp["meta_tokens"]), "lng": f(inp["ln_in_g"]), "lnb": f(inp["ln_in_b"]),
        "w_in": f(inp["w_in"][:depth]), "w_qb": f(np.asarray(inp["w_q_b"])[:depth][:, :, qperm]),
        "w_kvb": f(np.asarray(inp["w_kv_b"])[:depth][:, :, kperm]), "w_out": f(inp["w_out"][:depth]),
        "w_rt": f(inp["w_router"]), "rbias": f(inp["router_bias"]),
        "w_gate": f(inp["w_gate"][:depth]), "w_up": f(inp["w_up"][:depth]), "w_down": f(inp["w_down"][:depth]),
        "cols": cols, "ident": np.eye(128, dtype=np.float32), "rmat": _rmats(), "sel": sel.reshape(NE, NE * 128),
    }


def kernel(**inputs):
    SPC = SPC_DEFAULT
    ncores = 16 // SPC
    x = np.asarray(inputs["x"], dtype=np.float32)
    shared = _prep_shared(inputs, DEPTH)
    tA, tB = _rope_tables(SPC)
    shared["tabA"], shared["tabB"] = tA, tB
    nc = build_nc(SPC, DEPTH)
    in_maps = []
    for c in range(ncores):
        m = dict(shared)
        m["x"] = np.ascontiguousarray(x[c * SPC:(c + 1) * SPC])
        in_maps.append(m)
    res = run_bass_kernel_spmd(nc, in_maps, core_ids=list(range(ncores)))
    return np.concatenate([r["out"] for r in res.results], axis=0).astype(np.float32)
```

```python
import math
from contextlib import ExitStack
import numpy as np
import concourse.bass as bass
import concourse.mybir as mybir
from concourse.bass_utils import run_bass_kernel_spmd

F32 = mybir.dt.float32
BF16 = mybir.dt.bfloat16
AF = mybir.ActivationFunctionType
ALU = mybir.AluOpType
AX = mybir.AxisListType

D = 2048
SEQ = 2048
NMETA = 16
T = SEQ + NMETA
DEPTH = 4
INC = 2368
EPS = 1e-6
ALPHA = (2.0 * DEPTH) ** 0.25
MLA_SCALE = 1.0 / math.sqrt(192.0)
GQA_SCALE = 1.0 / math.sqrt(128.0)
NE = 16
FF = 1024
NCOL = 88
C_GQ, C_GKV, C_GQQ, C_GQK, C_GOA, C_GOB, C_L1G, C_L1B, C_L2G, C_L2B = 0, 4, 6, 7, 8, 16, 24, 40, 56, 72
NEG = -1.0e30

SPC_DEFAULT = 2


class St:
    def __init__(self, eng, sem, own=True):
        self.eng, self.sem, self.n, self.seen, self.own = eng, sem, 0, {}, own


class DSem:
    def __init__(self, sem):
        self.sem, self.cnt = sem, 0


class TB:
    def __init__(self, name, d=None):
        self.name, self.w, self.r, self.d = name, None, {}, d


class K:
    def __init__(self, nc, es):
        self.nc = nc
        sems = [es.enter_context(nc.semaphore(f"s{i}")) for i in range(40)]
        self.PE = St(nc.tensor, sems[0], own=False)
        self.ACT = St(nc.scalar, sems[1])
        self.DVE = St(nc.vector, sems[2])
        self.POOL = St(nc.gpsimd, sems[3])
        self.SP = St(nc.sync, sems[4])
        self.streams = [self.PE, self.ACT, self.DVE, self.POOL, self.SP]
        self.dpool = [DSem(s) for s in sems[5:30]]
        self.ppool = [DSem(s) for s in sems[30:]]
        self.dnext = 0
        self.pnext = 0
        self.pes = None

    def _wait(self, st, ev):
        if ev is None:
            return
        if ev[0] == "d":
            sem, val = ev[1].sem, 16 * ev[1].cnt
        else:
            sem, val = ev[1], ev[2]
            if sem is st.sem and not st.own:
                return
        if st.seen.get(sem.num, 0) >= val:
            return
        st.eng.wait_ge(sem, val)
        st.seen[sem.num] = val

    def dep(self, st, reads, writes):
        for b in reads:
            self._wait(st, b.w)
        for b in writes:
            self._wait(st, b.w)
            for ev in list(b.r.values()):
                self._wait(st, ev)

    def fin(self, ev, key, reads, writes):
        for b in reads:
            b.r[key] = ev
        for b in writes:
            b.w = ev
            b.r = {}

    def op(self, st, reads, writes, fn):
        self.dep(st, reads, writes)
        ins = fn(st.eng)
        st.n += 1
        ins.then_inc(st.sem, 1)
        self.fin(("c", st.sem, st.n), st.sem.num, reads, writes)

    def dma(self, q, out, in_, reads, writes, d):
        self.dep(q, reads, writes)
        ins = q.eng.dma_start(out=out, in_=in_)
        d.cnt += 1
        ins.then_inc(d.sem, 16)
        self.fin(("d", d), d.sem.num, reads, writes)

    def begin(self):
        self.pes = ExitStack()
        self.dnext = 0
        self.pnext = 0

    def sb(self, name, shape, dt, dma=False):
        self.uid = getattr(self, "uid", 0) + 1
        t = self.pes.enter_context(self.nc.sbuf_tensor(f"{name}_u{self.uid}", shape, dt))
        d = None
        if dma == "pool":
            d = self.ppool[self.pnext]
            self.pnext += 1
        elif dma:
            d = self.dpool[self.dnext]
            self.dnext += 1
        return t, TB(name, d)

    def barrier(self):
        evs = [("c", s.sem, s.n) for s in self.streams if s.n > 0]
        evs += [("d", d) for d in self.dpool + self.ppool if d.cnt > 0]
        for s in self.streams:
            for ev in evs:
                self._wait(s, ev)

    def end(self):
        self.barrier()
        self.pes.close()
        self.pes = None


def build_nc(SPC=SPC_DEFAULT, depth=DEPTH, dbg=False):
    nc = bass.Bass("TRN2", target_bir_lowering=False)
    NR = SPC * SEQ
    NM = SPC * NMETA
    NT = NR + NM

    def din(name, shape, dt=F32):
        return nc.dram_tensor(name, list(shape), dt, kind="ExternalInput").ap()

    x = din("x", [SPC, SEQ, D])
    meta = din("meta", [NMETA, D])
    lng = din("lng", [D])
    lnb = din("lnb", [D])
    w_in = din("w_in", [depth, D, INC])
    w_qb = din("w_qb", [depth, 512, 1536])
    w_kvb = din("w_kvb", [depth, 256, 2048])
    w_out = din("w_out", [depth, D, D])
    w_rt = din("w_rt", [D, NE])
    rbias = din("rbias", [NE])
    w_gate = din("w_gate", [depth, NE, D, FF])
    w_up = din("w_up", [depth, NE, D, FF])
    w_down = din("w_down", [depth, NE, FF, D])
    cols_d = din("cols", [128, depth * NCOL])
    ident_d = din("ident", [128, 128])
    rmat_d = din("rmat", [2, 128, 128])
    sel_d = din("sel", [NE, NE * 128])
    tabA = din("tabA", [2, 128, NT])
    tabB = din("tabB", [2, 128, NT])
    out = nc.dram_tensor("out", [SPC, SEQ, D], F32, kind="ExternalOutput").ap()

    okind = "ExternalOutput" if dbg else "Internal"

    def dscr(name, shape, dt):
        return nc.dram_tensor(name, list(shape), dt, kind=okind).ap()

    HT32 = dscr("HT32", [D, NT], F32)
    HTb = dscr("HTb", [D, NT], BF16)
    QaT = dscr("QaT", [8, 128, NT], BF16)
    QrT = dscr("QrT", [4, 128, NT], BF16)
    KaT = dscr("KaT", [8, 128, NT], BF16)
    KrT = dscr("KrT", [64, NT], BF16)
    Va = dscr("Va", [NT, 1024], BF16)
    GQT = dscr("GQT", [8, 128, NT], BF16)
    GKT = dscr("GKT", [2, 128, NT], BF16)
    GV = dscr("GV", [NT, 256], BF16)
    OT = dscr("OT", [D, NT], F32)

    def fm(ap2d):
        return ap2d.rearrange("(kc p) t -> p kc t", p=128)

    with ExitStack() as es:
        k = K(nc, es)
        PE, ACT, DVE, POOL, SP = k.PE, k.ACT, k.DVE, k.POOL, k.SP
        ps = [es.enter_context(nc.psum_tensor(f"ps{i}", [128, 512], F32)) for i in range(8)]
        PT = [TB(f"ps{i}") for i in range(8)]

        def gsb(name, shape, dt):
            return es.enter_context(nc.sbuf_tensor("g_" + name, shape, dt))

        gd = k.dpool.pop()
        ident = gsb("ident", [128, 128], F32)
        ones_f = gsb("ones_f", [128, 128], F32)
        ones_b = gsb("ones_b", [128, 128], BF16)
        rmat = gsb("rmat", [128, 2, 128], BF16)
        epsc = gsb("epsc", [128, 1], F32)
        eps2c = gsb("eps2c", [128, 1], F32)
        sel = gsb("sel", [NE, NE * 128], F32)
        cols = gsb("cols", [128, depth * NCOL], F32)
        rb_bc = gsb("rb_bc", [128, NE], F32)
        wrt = gsb("wrt", [128, 16, NE], F32)
        CONST = TB("const", gd)
        k.dma(SP, ident[:], ident_d[:, :], [], [CONST], gd)
        k.dma(SP, sel[:], sel_d[:, :], [], [CONST], gd)
        k.dma(SP, cols[:], cols_d[:, :], [], [CONST], gd)
        k.dma(SP, rb_bc[:], rbias.partition_broadcast(128), [], [CONST], gd)
        k.dma(SP, wrt[:], w_rt.rearrange("(kc p) e -> p kc e", p=128), [], [CONST], gd)
        gdp = k.ppool.pop()
        CONSTP = TB("constp", gdp)
        k.dma(POOL, rmat[:], rmat_d.rearrange("c p m -> p c m"), [], [CONSTP], gdp)
        CM = TB("constmem")
        k.op(DVE, [], [CM], lambda e: e.memset(ones_f[:], 1.0))
        k.op(DVE, [], [CM], lambda e: e.memset(ones_b[:], 1.0))
        k.op(DVE, [], [CM], lambda e: e.memset(epsc[:], EPS))
        k.op(DVE, [], [CM], lambda e: e.memset(eps2c[:], EPS / (ALPHA * ALPHA)))
        CR = [CONST, CONSTP, CM]

        def mm(pt, out_ap, pairs, reads, start=True, stop=True):
            def f(e):
                ins = None
                n = len(pairs)
                for i, (l, r) in enumerate(pairs):
                    ins = e.matmul(out_ap, l, r, start=(start and i == 0), stop=(stop and i == n - 1))
                return ins
            k.op(PE, reads + CR, [pt], f)

        def feature_blocks():
            bl = [(i * 512, 512) for i in range(NR // 512)]
            bl.append((NR, NM))
            return bl

        def rsqrt_from(pt, ps_ap, out_tb, out_ap, scale, eps_ap):
            k.op(ACT, [pt] + CR, [out_tb], lambda e: e.activation(out=out_ap, in_=ps_ap, func=AF.Sqrt, bias=eps_ap, scale=scale))
            k.op(DVE, [out_tb], [out_tb], lambda e: e.reciprocal(out=out_ap, in_=out_ap))

        def phase0():
            k.begin()
            xt = [k.sb(f"xt{i}", [128, D], F32, dma=True) for i in range(2)]
            hbs = [k.sb(f"hb{i}", [128, D], F32) for i in range(2)]
            gbc, GB = k.sb("gbc", [128, D], F32, dma=True)
            bbc, BB = k.sb("bbc", [128, D], F32, dma=True)
            stf, STF = k.sb("stf", [128, 16, 512], F32, dma=True)
            stb, STB = k.sb("stb", [128, 16, 512], BF16, dma=True)
            sms = [k.sb(f"sm{i}", [128, 8], F32) for i in range(2)]
            k.dma(SP, gbc[:], lng.partition_broadcast(128), [], [GB], GB.d)
            k.dma(SP, bbc[:], lnb.partition_broadcast(128), [], [BB], BB.d)
            it = 0
            import os as _os
            for (t0, n) in feature_blocks():
                if _os.environ.get("P0_SKIP_META") and n < 128:
                    continue
                ntile = (n + 127) // 128
                for j in range(ntile):
                    p = min(128, n - j * 128)
                    xs, XS = xt[it % 2]
                    hb, HB = hbs[it % 2]
                    sm, SM = sms[it % 2]
                    it += 1
                    if t0 < NR:
                        s, s0 = divmod(t0 + j * 128, SEQ)
                        k.dma(SP, xs[0:p, :], x[s, s0:s0 + p, :], [], [XS], XS.d)
                    else:
                        for s in range(SPC):
                            k.dma(SP, xs[16 * s:16 * s + 16, :], meta[:, :], [], [XS], XS.d)
                    _step = int(_os.environ.get("P0_STEP", "99"))
                    if _step <= 1:
                        k.end(); return
                    k.op(DVE, [], [SM], lambda e: e.memset(sm[0:p, 0:2], 0.0))
                    k.op(ACT, [XS], [HB, SM], lambda e: e.activation(out=hb[0:p, :], in_=xs[0:p, :], func=AF.Identity, accum_out=sm[0:p, 0:1]))
                    k.op(ACT, [XS], [HB, SM], lambda e: e.activation(out=hb[0:p, :], in_=xs[0:p, :], func=AF.Square, accum_out=sm[0:p, 1:2]))
                    if _step <= 2:
                        k.end(); return
                    k.op(DVE, [SM], [SM], lambda e: e.tensor_scalar(out=sm[0:p, 2:3], in0=sm[0:p, 0:1], scalar1=1.0 / D, scalar2=None, op0=ALU.mult))
                    k.op(DVE, [SM], [SM], lambda e: e.tensor_tensor(out=sm[0:p, 3:4], in0=sm[0:p, 2:3], in1=sm[0:p, 2:3], op=ALU.mult))
                    k.op(DVE, [SM], [SM], lambda e: e.scalar_tensor_tensor(out=sm[0:p, 4:5], in0=sm[0:p, 1:2], scalar=1.0 / D, in1=sm[0:p, 3:4], op0=ALU.mult, op1=ALU.subtract))
                    k.op(ACT, [SM] + CR, [SM], lambda e: e.activation(out=sm[0:p, 5:6], in_=sm[0:p, 4:5], func=AF.Sqrt, bias=epsc[0:p, :], scale=1.0))
                    k.op(DVE, [SM], [SM], lambda e: e.reciprocal(out=sm[0:p, 5:6], in_=sm[0:p, 5:6]))
                    k.op(DVE, [SM], [SM], lambda e: e.scalar_tensor_tensor(out=sm[0:p, 6:7], in0=sm[0:p, 2:3], scalar=-1.0, in1=sm[0:p, 5:6], op0=ALU.mult, op1=ALU.mult))
                    if _step <= 3:
                        k.end(); return
                    k.op(ACT, [XS, SM], [HB], lambda e: e.activation(out=hb[0:p, :], in_=xs[0:p, :], func=AF.Identity, bias=sm[0:p, 6:7], scale=sm[0:p, 5:6]))
                    k.op(DVE, [HB, GB], [HB], lambda e: e.tensor_tensor(out=hb[0:p, :], in0=hb[0:p, :], in1=gbc[0:p, :], op=ALU.mult))
                    k.op(DVE, [HB, BB], [HB], lambda e: e.tensor_tensor(out=hb[0:p, :], in0=hb[0:p, :], in1=bbc[0:p, :], op=ALU.add))
                    if _step <= 4:
                        k.end(); return
                    for b4 in range(4):
                        def tr(e, b4=b4):
                            ins = None
                            for q in range(4):
                                kc = b4 * 4 + q
                                ins = e.transpose(ps[b4][:, q * 128:q * 128 + p], hb[0:p, kc * 128:(kc + 1) * 128], ident[0:p, 0:p])
                            return ins
                        k.op(PE, [HB] + CR, [PT[b4]], tr)
                        for q in range(4):
                            kc = b4 * 4 + q
                            src = ps[b4][:, q * 128:q * 128 + p]
                            k.op(ACT, [PT[b4]], [STF], lambda e, src=src, kc=kc: e.activation(out=stf[:, kc, j * 128:j * 128 + p], in_=src, func=AF.Identity))
                            k.op(DVE, [STF], [STB], lambda e, kc=kc: e.tensor_copy(out=stb[:, kc, j * 128:j * 128 + p], in_=stf[:, kc, j * 128:j * 128 + p]))
                    if _step <= 5:
                        k.end(); return
                if _step <= 6:
                    k.end(); return
                k.dma(SP, fm(HT32[:, t0:t0 + n]), stf[:, :, 0:n], [STF], [], STF.d)
                k.dma(SP, fm(HTb[:, t0:t0 + n]), stb[:, :, 0:n], [STB], [], STB.d)
            k.end()

        def rope_unit(P, n, xg, XG, ri, tab, TAB, t1, T1, t2, T2, out_tb, out_ap, pr, rr=None, RR=None):
            mm(PT[pr], ps[pr][0:P, 0:n], [(rmat[0:P, ri, 0:P], xg[0:P, 0:n])], [XG])
            k.op(POOL, [XG, TAB], [T1], lambda e: e.tensor_tensor(out=t1[0:P, 0:n], in0=xg[0:P, 0:n], in1=tab[0:P, 0, 0:n], op=ALU.mult))
            k.op(DVE, [PT[pr], TAB], [T2], lambda e: e.tensor_tensor(out=t2[0:P, 0:n], in0=ps[pr][0:P, 0:n], in1=tab[0:P, 1, 0:n], op=ALU.mult))
            if rr is None:
                k.op(POOL, [T1, T2], [out_tb], lambda e: e.tensor_tensor(out=out_ap, in0=t1[0:P, 0:n], in1=t2[0:P, 0:n], op=ALU.add))
            else:
                k.op(POOL, [T1, T2], [T1], lambda e: e.tensor_tensor(out=t1[0:P, 0:n], in0=t1[0:P, 0:n], in1=t2[0:P, 0:n], op=ALU.add))
                k.op(DVE, [T1, RR], [out_tb], lambda e: e.tensor_tensor(out=out_ap, in0=t1[0:P, 0:n], in1=rr[0:P, 0:n], op=ALU.mult))

        def phaseA1(l):
            k.begin()
            co = l * NCOL
            wA, WA = k.sb("wA", [128, 16, 832], BF16, dma="pool")
            wq, _ = k.sb("wq", [128, 4, 1536], BF16)
            wkv, _ = k.sb("wkv", [128, 2, 2048], BF16)
            k.dma(POOL, wA[:], fm(w_in[l, :, 0:832]), [], [WA], WA.d)
            k.dma(POOL, wq[:], fm(w_qb[l]), [], [WA], WA.d)
            k.dma(POOL, wkv[:], fm(w_kvb[l]), [], [WA], WA.d)
            hts = [k.sb(f"hT{i}", [128, 16, 512], BF16, dma=True) for i in range(2)]
            tas = [k.sb(f"ta{i}", [128, 2, 512], F32, dma=True) for i in range(2)]
            cqg, CQG = k.sb("cqg", [128, 4, 512], BF16)
            ckg, CKG = k.sb("ckg", [128, 2, 512], BF16)
            sqk, SQK = k.sb("sqk", [128, 2, 512], BF16)
            sqb = [k.sb(f"sqb{i}", [128, 512], BF16) for i in range(2)]
            xr = [k.sb(f"xr{i}", [128, 512], BF16) for i in range(2)]
            rq, RQ = k.sb("rq", [128, 512], F32)
            rkv, RKV = k.sb("rkv", [128, 512], F32)
            rvc, RVC = k.sb("rvc", [128, 1], F32)
            t1s = [k.sb(f"t1{i}", [128, 512], F32) for i in range(2)]
            t2s = [k.sb(f"t2{i}", [128, 512], F32) for i in range(2)]
            qa, QA = k.sb("qa_st", [128, 8, 512], BF16, dma=True)
            qr, QR = k.sb("qr_st", [128, 4, 512], BF16, dma=True)
            ka, KA = k.sb("ka_st", [128, 8, 512], BF16, dma=True)
            kr, KR = k.sb("kr_st", [64, 512], BF16, dma=True)
            va, VA = k.sb("va_st", [128, 4, 1024], BF16, dma=True)
            ctr = [0]

            def nx():
                ctr[0] += 1
                return ctr[0]
            for bi, (t0, n) in enumerate(feature_blocks()):
                hT, HTB_ = hts[bi % 2]
                ta, TA = tas[bi % 2]
                k.dma(SP, hT[:, :, 0:n], fm(HTb[:, t0:t0 + n]), [], [HTB_], HTB_.d)
                k.dma(SP, ta[:, :, 0:n], tabA[:, :, t0:t0 + n].rearrange("c p t -> p c t"), [], [TA], TA.d)
                for c in range(4):
                    u = nx() % 2
                    mm(PT[u], ps[u][:, 0:n], [(wA[:, kc, c * 128:(c + 1) * 128], hT[:, kc, 0:n]) for kc in range(16)], [WA, HTB_])
                    sb_, SB_ = sqb[c % 2]
                    k.op(ACT, [PT[u]], [SB_], lambda e, u=u, sb_=sb_: e.activation(out=sb_[:, 0:n], in_=ps[u][:, 0:n], func=AF.Square))
                    k.op(ACT, [PT[u]] + CR, [CQG], lambda e, u=u, c=c: e.activation(out=cqg[:, c, 0:n], in_=ps[u][:, 0:n], func=AF.Identity, scale=cols[:, co + C_GQ + c:co + C_GQ + c + 1]))
                    mm(PT[2], ps[2][:, 0:n], [(ones_b[:, :], sb_[:, 0:n])], [SB_], start=(c == 0), stop=(c == 3))
                rsqrt_from(PT[2], ps[2][:, 0:n], RQ, rq[:, 0:n], 1.0 / 512, epsc[:, :])
                for c in range(2):
                    u = nx() % 2
                    mm(PT[u], ps[u][:, 0:n], [(wA[:, kc, 512 + c * 128:512 + (c + 1) * 128], hT[:, kc, 0:n]) for kc in range(16)], [WA, HTB_])
                    k.op(ACT, [PT[u]], [SQK], lambda e, u=u, c=c: e.activation(out=sqk[:, c, 0:n], in_=ps[u][:, 0:n], func=AF.Square))
                    k.op(ACT, [PT[u]] + CR, [CKG], lambda e, u=u, c=c: e.activation(out=ckg[:, c, 0:n], in_=ps[u][:, 0:n], func=AF.Identity, scale=cols[:, co + C_GKV + c:co + C_GKV + c + 1]))
                    mm(PT[3], ps[3][:, 0:n], [(ones_b[:, :], sqk[:, c, 0:n])], [SQK], start=(c == 0), stop=(c == 1))
                rsqrt_from(PT[3], ps[3][:, 0:n], RKV, rkv[:, 0:n], 1.0 / 256, epsc[:, :])
                for h in range(8):
                    u = nx() % 2
                    mm(PT[u], ps[u][:, 0:n], [(wq[:, kc, h * 128:(h + 1) * 128], cqg[:, kc, 0:n]) for kc in range(4)], [WA, CQG])
                    k.op(DVE, [PT[u], RQ], [QA], lambda e, u=u, h=h: e.tensor_tensor(out=qa[:, h, 0:n], in0=ps[u][:, 0:n], in1=rq[:, 0:n], op=ALU.mult))
                for j in range(4):
                    u = nx() % 2
                    mm(PT[u], ps[u][:, 0:n], [(wq[:, kc, 1024 + j * 128:1024 + (j + 1) * 128], cqg[:, kc, 0:n]) for kc in range(4)], [WA, CQG])
                    xr_, XR_ = xr[j % 2]
                    k.op(DVE, [PT[u], RQ], [XR_], lambda e, u=u, xr_=xr_: e.tensor_tensor(out=xr_[:, 0:n], in0=ps[u][:, 0:n], in1=rq[:, 0:n], op=ALU.mult))
                    t1, T1 = t1s[j % 2]
                    t2, T2 = t2s[j % 2]
                    rope_unit(128, n, xr_, XR_, 0, ta, TA, t1, T1, t2, T2, QR, qr[:, j, 0:n], 4 + j % 2)
                for h in range(8):
                    u = nx() % 2
                    mm(PT[u], ps[u][:, 0:n], [(wkv[:, kc, h * 128:(h + 1) * 128], ckg[:, kc, 0:n]) for kc in range(2)], [WA, CKG])
                    k.op(DVE, [PT[u], RKV], [KA], lambda e, u=u, h=h: e.tensor_tensor(out=ka[:, h, 0:n], in0=ps[u][:, 0:n], in1=rkv[:, 0:n], op=ALU.mult))
                u = nx() % 2
                mm(PT[u], ps[u][0:64, 0:n], [(wA[:, kc, 768:832], hT[:, kc, 0:n]) for kc in range(16)], [WA, HTB_])
                xr_, XR_ = xr[0]
                k.op(ACT, [PT[u]], [XR_], lambda e, u=u, xr_=xr_: e.activation(out=xr_[0:64, 0:n], in_=ps[u][0:64, 0:n], func=AF.Identity))
                rope_unit(64, n, xr_, XR_, 0, ta, TA, t1s[0][0], t1s[0][1], t2s[0][0], t2s[0][1], KR, kr[0:64, 0:n], 4)
                ntile = (n + 127) // 128
                for j in range(ntile):
                    p = min(128, n - j * 128)
                    mm(PT[6], ps[6][0:p, 0:1], [(sqk[:, c, j * 128:j * 128 + p], ones_b[:, 0:1]) for c in range(2)], [SQK])
                    k.op(ACT, [PT[6]] + CR, [RVC], lambda e: e.activation(out=rvc[0:p, :], in_=ps[6][0:p, 0:1], func=AF.Sqrt, bias=epsc[0:p, :], scale=1.0 / 256))
                    k.op(DVE, [RVC], [RVC], lambda e: e.reciprocal(out=rvc[0:p, :], in_=rvc[0:p, :]))
                    for hf in range(2):
                        u = nx() % 2
                        mm(PT[u], ps[u][0:p, :], [(ckg[:, c, j * 128:j * 128 + p], wkv[:, c, 1024 + hf * 512:1024 + (hf + 1) * 512]) for c in range(2)], [WA, CKG])
                        k.op(ACT, [PT[u], RVC], [VA], lambda e, u=u, hf=hf: e.activation(out=va[0:p, j, hf * 512:(hf + 1) * 512], in_=ps[u][0:p, :], func=AF.Identity, scale=rvc[0:p, 0:1]))
                k.dma(SP, QaT[:, :, t0:t0 + n].rearrange("h p t -> p h t"), qa[:, :, 0:n], [QA], [], QA.d)
                k.dma(SP, QrT[:, :, t0:t0 + n].rearrange("h p t -> p h t"), qr[:, :, 0:n], [QR], [], QR.d)
                k.dma(SP, KaT[:, :, t0:t0 + n].rearrange("h p t -> p h t"), ka[:, :, 0:n], [KA], [], KA.d)
                k.dma(SP, KrT[:, t0:t0 + n], kr[0:64, 0:n], [KR], [], KR.d)
                if n % 128 == 0:
                    k.dma(SP, Va[t0:t0 + n, :].rearrange("(j p) f -> p j f", p=128), va[:, 0:n // 128, :], [VA], [], VA.d)
                else:
                    k.dma(SP, Va[t0:t0 + n, :], va[0:n, 0, :], [VA], [], VA.d)
            k.end()

        def phaseA2(l):
            k.begin()
            co = l * NCOL
            wB, WB = k.sb("wB", [128, 16, 1536], BF16, dma="pool")
            k.dma(POOL, wB[:], fm(w_in[l, :, 832:2368]), [], [WB], WB.d)
            hts = [k.sb(f"hT{i}", [128, 16, 512], BF16, dma=True) for i in range(2)]
            tbs = [k.sb(f"tb{i}", [128, 2, 512], F32, dma=True) for i in range(2)]
            sqb = [k.sb(f"sqb{i}", [128, 512], BF16) for i in range(2)]
            xg = [k.sb(f"xg{i}", [128, 512], BF16) for i in range(2)]
            rrs = [k.sb(f"rr{i}", [128, 512], F32) for i in range(2)]
            t1s = [k.sb(f"t1{i}", [128, 512], F32) for i in range(2)]
            t2s = [k.sb(f"t2{i}", [128, 512], F32) for i in range(2)]
            gq, GQ = k.sb("gq_st", [128, 10, 512], BF16, dma=True)
            gv, GVS = k.sb("gv_st", [128, 4, 256], BF16, dma=True)
            for bi, (t0, n) in enumerate(feature_blocks()):
                hT, HTB_ = hts[bi % 2]
                tb, TBL = tbs[bi % 2]
                k.dma(SP, hT[:, :, 0:n], fm(HTb[:, t0:t0 + n]), [], [HTB_], HTB_.d)
                k.dma(SP, tb[:, :, 0:n], tabB[:, :, t0:t0 + n].rearrange("c p t -> p c t"), [], [TBL], TBL.d)
                for h in range(10):
                    u = h % 2
                    gcol = co + (C_GQQ if h < 8 else C_GQK)
                    mm(PT[u], ps[u][:, 0:n], [(wB[:, kc, h * 128:(h + 1) * 128], hT[:, kc, 0:n]) for kc in range(16)], [WB, HTB_])
                    sb_, SB_ = sqb[u]
                    xg_, XG_ = xg[u]
                    rr, RR = rrs[u]
                    k.op(ACT, [PT[u]], [SB_], lambda e, u=u, sb_=sb_: e.activation(out=sb_[:, 0:n], in_=ps[u][:, 0:n], func=AF.Square))
                    k.op(ACT, [PT[u]] + CR, [XG_], lambda e, u=u, xg_=xg_, gcol=gcol: e.activation(out=xg_[:, 0:n], in_=ps[u][:, 0:n], func=AF.Identity, scale=cols[:, gcol:gcol + 1]))
                    mm(PT[2 + u], ps[2 + u][:, 0:n], [(ones_b[:, :], sb_[:, 0:n])], [SB_])
                    rsqrt_from(PT[2 + u], ps[2 + u][:, 0:n], RR, rr[:, 0:n], 1.0 / 128, epsc[:, :])
                    rope_unit(128, n, xg_, XG_, 1, tb, TBL, t1s[u][0], t1s[u][1], t2s[u][0], t2s[u][1], GQ, gq[:, h, 0:n], 4 + u, rr, RR)
                ntile = (n + 127) // 128
                for j in range(ntile):
                    p = min(128, n - j * 128)
                    u = 6 + j % 2
                    mm(PT[u], ps[u][0:p, 0:256], [(hT[:, kc, j * 128:j * 128 + p], wB[:, kc, 1280:1536]) for kc in range(16)], [WB, HTB_])
                    k.op(ACT, [PT[u]], [GVS], lambda e, u=u: e.activation(out=gv[0:p, j, :], in_=ps[u][0:p, 0:256], func=AF.Identity))
                k.dma(SP, GQT[:, :, t0:t0 + n].rearrange("h p t -> p h t"), gq[:, 0:8, 0:n], [GQ], [], GQ.d)
                k.dma(SP, GKT[:, :, t0:t0 + n].rearrange("h p t -> p h t"), gq[:, 8:10, 0:n], [GQ], [], GQ.d)
                if n % 128 == 0:
                    k.dma(SP, GV[t0:t0 + n, :].rearrange("(j p) f -> p j f", p=128), gv[:, 0:n // 128, :], [GVS], [], GVS.d)
                else:
                    k.dma(SP, GV[t0:t0 + n, :], gv[0:n, 0, :], [GVS], [], GVS.d)
            k.end()

        def phaseB1(l):
            k.begin()
            kaS, KV = k.sb("kaS", [128, 8, T], BF16, dma=True)
            kr2, _ = k.sb("kr2", [128, T], BF16)
            gkS, _ = k.sb("gkS", [128, 2, T], BF16)
            vaS, _ = k.sb("vaS", [128, 17, 1024], BF16)
            gvS, _ = k.sb("gvS", [128, 17, 256], BF16)
            qsl = []
            for i in range(2):
                a_, A_ = k.sb(f"qa{i}", [128, 8, 512], BF16, dma=True)
                r_, _ = k.sb(f"qr{i}", [128, 4, 512], BF16)
                g_, _ = k.sb(f"gq{i}", [128, 8, 512], BF16)
                qsl.append((a_, r_, g_, A_))
            pts = [k.sb(f"pt{i}", [128, 512], BF16) for i in range(3)]
            rl, RL = k.sb("rl", [128, 512], F32)
            ots = [k.sb(f"ot{i}", [128, 512], F32, dma=True) for i in range(4)]
            qi = 0
            oi = 0
            hcount = 0
            for s in range(SPC):
                r0, m0 = s * SEQ, NR + s * NMETA
                for (dst, src) in [
                    (kaS[:, :, 0:SEQ], KaT[:, :, r0:r0 + SEQ].rearrange("h p t -> p h t")),
                    (kaS[:, :, SEQ:T], KaT[:, :, m0:m0 + NMETA].rearrange("h p t -> p h t")),
                    (kr2[0:64, 0:SEQ], KrT[:, r0:r0 + SEQ]), (kr2[64:128, 0:SEQ], KrT[:, r0:r0 + SEQ]),
                    (kr2[0:64, SEQ:T], KrT[:, m0:m0 + NMETA]), (kr2[64:128, SEQ:T], KrT[:, m0:m0 + NMETA]),
                    (gkS[:, :, 0:SEQ], GKT[:, :, r0:r0 + SEQ].rearrange("h p t -> p h t")),
                    (gkS[:, :, SEQ:T], GKT[:, :, m0:m0 + NMETA].rearrange("h p t -> p h t")),
                    (vaS[:, 0:16, :], Va[r0:r0 + SEQ, :].rearrange("(j p) f -> p j f", p=128)),
                    (vaS[0:16, 16, :], Va[m0:m0 + NMETA, :]),
                    (gvS[:, 0:16, :], GV[r0:r0 + SEQ, :].rearrange("(j p) f -> p j f", p=128)),
                    (gvS[0:16, 16, :], GV[m0:m0 + NMETA, :]),
                ]:
                    k.dma(SP, dst, src, [], [KV], KV.d)
                qblocks = [(r0 + i * 512, 512) for i in range(4)] + [(m0, NMETA)]
                for (t0, nq) in qblocks:
                    qa_, qr_, gq_, QS = qsl[qi % 2]
                    qi += 1
                    k.dma(SP, qa_[:, :, 0:nq], QaT[:, :, t0:t0 + nq].rearrange("h p t -> p h t"), [], [QS], QS.d)
                    k.dma(SP, qr_[:, :, 0:nq], QrT[:, :, t0:t0 + nq].rearrange("h p t -> p h t"), [], [QS], QS.d)
                    k.dma(SP, gq_[:, :, 0:nq], GQT[:, :, t0:t0 + nq].rearrange("h p t -> p h t"), [], [QS], QS.d)
                    for hh in range(16):
                        o = hcount % 2
                        hcount += 1
                        PO, PL = PT[3 + o], PT[5 + o]
                        pso, psl = ps[3 + o], ps[5 + o]

                        def emitS(kt, hh=hh):
                            k0 = kt * 128
                            kp = 128 if kt < 16 else NMETA
                            i = kt % 3
                            if hh < 8:
                                pb = 64 * (hh % 2)
                                pairs = [(kaS[:, hh, k0:k0 + kp], qa_[:, hh, 0:nq]),
                                         (kr2[pb:pb + 64, k0:k0 + kp], qr_[pb:pb + 64, hh // 2, 0:nq])]
                            else:
                                g = hh - 8
                                pairs = [(gkS[:, g // 4, k0:k0 + kp], gq_[:, g, 0:nq])]
                            mm(PT[i], ps[i][0:kp, 0:nq], pairs, [KV, QS])

                        def emitE(kt, hh=hh):
                            kp = 128 if kt < 16 else NMETA
                            i = kt % 3
                            pt_, PT_ = pts[i]
                            sc = MLA_SCALE if hh < 8 else GQA_SCALE
                            k.op(ACT, [PT[i]], [PT_], lambda e: e.activation(out=pt_[0:kp, 0:nq], in_=ps[i][0:kp, 0:nq], func=AF.Exp, scale=sc))

                        def emitO(kt, hh=hh):
                            kp = 128 if kt < 16 else NMETA
                            i = kt % 3
                            pt_, PT_ = pts[i]
                            if hh < 8:
                                vv = vaS[0:kp, kt, hh * 128:(hh + 1) * 128]
                            else:
                                g = (hh - 8) // 4
                                vv = gvS[0:kp, kt, g * 128:(g + 1) * 128]
                            mm(PO, pso[:, 0:nq], [(vv, pt_[0:kp, 0:nq])], [KV, PT_], start=(kt == 0), stop=(kt == 16))
                            mm(PL, psl[:, 0:nq], [(ones_b[0:kp, :], pt_[0:kp, 0:nq])], [PT_], start=(kt == 0), stop=(kt == 16))
                        emitS(0)
                        emitS(1)
                        for kt in range(17):
                            emitE(kt)
                            if kt + 2 < 17:
                                emitS(kt + 2)
                            emitO(kt)
                        ot_, OT_ = ots[oi % 4]
                        oi += 1
                        k.op(DVE, [PL], [RL], lambda e: e.reciprocal(out=rl[:, 0:nq], in_=psl[:, 0:nq]))
                        k.op(DVE, [PO, RL], [OT_], lambda e, ot_=ot_: e.tensor_tensor(out=ot_[:, 0:nq], in0=pso[:, 0:nq], in1=rl[:, 0:nq], op=ALU.mult))
                        k.dma(SP, OT[hh * 128:(hh + 1) * 128, t0:t0 + nq], ot_[:, 0:nq], [OT_], [], OT_.d)
            k.end()

        def ln_fm(n, z, Z, zb, ZB, gc, bc, eps_ap, tmps):
            (zsq, mean, MEAN, rstd, RSTD, nmr, NMR) = tmps
            mm(PT[6], ps[6][:, 0:n], [(ones_f[:, :], z(kc)) for kc in range(16)], list(Z))
            for kc in range(16):
                zs, ZS = zsq[kc % 2]
                k.op(ACT, [Z[kc]], [ZS], lambda e, zs=zs, kc=kc: e.activation(out=zs[:, 0:n], in_=z(kc), func=AF.Square))
                mm(PT[7], ps[7][:, 0:n], [(ones_f[:, :], zs[:, 0:n])], [ZS], start=(kc == 0), stop=(kc == 15))
            k.op(ACT, [PT[6]], [MEAN], lambda e: e.activation(out=mean[:, 0:n], in_=ps[6][:, 0:n], func=AF.Identity, scale=1.0 / D))
            k.op(DVE, [MEAN], [NMR], lambda e: e.tensor_tensor(out=nmr[:, 0:n], in0=mean[:, 0:n], in1=mean[:, 0:n], op=ALU.mult))
            k.op(DVE, [PT[7], NMR], [RSTD], lambda e: e.scalar_tensor_tensor(out=rstd[:, 0:n], in0=ps[7][:, 0:n], scalar=1.0 / D, in1=nmr[:, 0:n], op0=ALU.mult, op1=ALU.subtract))
            k.op(ACT, [RSTD] + CR, [RSTD], lambda e: e.activation(out=rstd[:, 0:n], in_=rstd[:, 0:n], func=AF.Sqrt, bias=eps_ap, scale=1.0))
            k.op(DVE, [RSTD], [RSTD], lambda e: e.reciprocal(out=rstd[:, 0:n], in_=rstd[:, 0:n]))
            k.op(DVE, [MEAN, RSTD], [NMR], lambda e: e.tensor_tensor(out=nmr[:, 0:n], in0=mean[:, 0:n], in1=rstd[:, 0:n], op=ALU.mult))
            for kc in range(16):
                k.op(DVE, [Z[kc], RSTD], [Z[kc]], lambda e, kc=kc: e.tensor_tensor(out=z(kc), in0=z(kc), in1=rstd[:, 0:n], op=ALU.mult))
                k.op(POOL, [Z[kc], NMR], [Z[kc]], lambda e, kc=kc: e.tensor_tensor(out=z(kc), in0=z(kc), in1=nmr[:, 0:n], op=ALU.subtract))
                k.op(ACT, [Z[kc]] + CR, [Z[kc]], lambda e, kc=kc: e.activation(out=z(kc), in_=z(kc), func=AF.Identity, bias=cols[:, bc + kc:bc + kc + 1], scale=cols[:, gc + kc:gc + kc + 1]))
            if zb is not None:
                for kc in range(16):
                    if kc % 2 == 0:
                        k.op(ACT, [Z[kc]], [ZB[kc]], lambda e, kc=kc: e.activation(out=zb(kc), in_=z(kc), func=AF.Identity))
                    else:
                        k.op(POOL, [Z[kc]], [ZB[kc]], lambda e, kc=kc: e.tensor_copy(out=zb(kc), in_=z(kc)))

        def ln_tmps():
            zsq = [k.sb(f"zsq{i}", [128, 512], F32) for i in range(2)]
            mean, MEAN = k.sb("mean", [128, 512], F32)
            rstd, RSTD = k.sb("rstd", [128, 512], F32)
            nmr, NMR = k.sb("nmr", [128, 512], F32)
            return (zsq, mean, MEAN, rstd, RSTD, nmr, NMR)

        def phaseB2(l):
            k.begin()
            co = l * NCOL
            wo, WO = k.sb("wo", [128, 16, D], BF16, dma="pool")
            k.dma(POOL, wo[:], fm(w_out[l]), [], [WO], WO.d)
            ot, OTB = k.sb("ot", [128, 16, 512], F32, dma=True)
            onb, ONB = k.sb("onb", [128, 16, 512], BF16)
            hz, HZ = k.sb("hz", [128, 16, 512], F32, dma=True)
            hzb, HZB = k.sb("hzb", [128, 16, 512], BF16, dma=True)
            sqb = [k.sb(f"sqb{i}", [128, 512], BF16) for i in range(2)]
            ra, RA = k.sb("ra", [128, 512], F32)
            rb, RB = k.sb("rb", [128, 512], F32)
            tmps = ln_tmps()
            OTc = [TB(f"otc{c}") for c in range(16)]
            ONc = [TB(f"onc{c}") for c in range(16)]
            HZc = [TB(f"hzc{c}") for c in range(16)]
            HBc = [TB(f"hbc{c}") for c in range(16)]
            for (t0, n) in feature_blocks():
                k.dma(SP, ot[:, :, 0:n], fm(OT[:, t0:t0 + n]), [], OTc, OTB.d)
                k.dma(SP, hz[:, :, 0:n], fm(HT32[:, t0:t0 + n]), [], HZc, HZ.d)
                for grp in range(2):
                    for c in range(8):
                        kc = grp * 8 + c
                        sb_, SB_ = sqb[kc % 2]
                        k.op(ACT, [OTc[kc]], [SB_], lambda e, sb_=sb_, kc=kc: e.activation(out=sb_[:, 0:n], in_=ot[:, kc, 0:n], func=AF.Square))
                        mm(PT[2 + grp], ps[2 + grp][:, 0:n], [(ones_b[:, :], sb_[:, 0:n])], [SB_], start=(c == 0), stop=(c == 7))
                rsqrt_from(PT[2], ps[2][:, 0:n], RA, ra[:, 0:n], 1.0 / 1024, epsc[:, :])
                rsqrt_from(PT[3], ps[3][:, 0:n], RB, rb[:, 0:n], 1.0 / 1024, epsc[:, :])
                for kc in range(16):
                    r_, R_ = (ra, RA) if kc < 8 else (rb, RB)
                    gcx = co + C_GOA + kc
                    k.op(DVE, [OTc[kc], R_] + CR, [ONc[kc]], lambda e, kc=kc, r_=r_, gcx=gcx: e.scalar_tensor_tensor(out=onb[:, kc, 0:n], in0=ot[:, kc, 0:n], scalar=cols[:, gcx:gcx + 1], in1=r_[:, 0:n], op0=ALU.mult, op1=ALU.mult))
                for fc in range(16):
                    u = fc % 2
                    mm(PT[u], ps[u][:, 0:n], [(wo[:, kc, fc * 128:(fc + 1) * 128], onb[:, kc, 0:n]) for kc in range(16)], [WO] + ONc)
                    k.op(DVE, [PT[u], HZc[fc]], [HZc[fc]], lambda e, u=u, fc=fc: e.scalar_tensor_tensor(out=hz[:, fc, 0:n], in0=hz[:, fc, 0:n], scalar=ALPHA, in1=ps[u][:, 0:n], op0=ALU.mult, op1=ALU.add))
                ln_fm(n, lambda kc: hz[:, kc, 0:n], HZc, lambda kc: hzb[:, kc, 0:n], HBc, co + C_L1G, co + C_L1B, epsc[:, :], tmps)
                k.dma(SP, fm(HT32[:, t0:t0 + n]), hz[:, :, 0:n], HZc, [], HZ.d)
                k.dma(SP, fm(HTb[:, t0:t0 + n]), hzb[:, :, 0:n], HBc, [], HZB.d)
            k.end()

        def phaseC(l, last):
            k.begin()
            co = l * NCOL
            TBM = 1024 + NM
            hT, HT_ = k.sb("h1T", [128, 16, TBM], BF16, dma=True)
            ya, YA = k.sb("yacc", [128, 16, TBM], F32, dma=True)
            he, HE = k.sb("heT", [128, 8, TBM], BF16)
            cbs = [k.sb(f"cbc{i}", [128, TBM], BF16) for i in range(2)]
            wg = [k.sb(f"wg{i}", [128, 16, 256], BF16, dma="pool") for i in range(2)]
            wu = [k.sb(f"wu{i}", [128, 16, 256], BF16, dma="pool") for i in range(2)]
            wd = [k.sb(f"wd{i}", [128, 8, 512], BF16, dma="pool") for i in range(2)]
            sgs = [k.sb(f"sg{i}", [128, 512], BF16) for i in range(3)]
            tts = [k.sb(f"tt{i}", [128, 512], BF16) for i in range(3)]
            cmbT, CMBT = k.sb("cmbT", [NE, TBM], F32)
            rt, RT = k.sb("rt", [128, 12, NE], F32)
            rs, RS = k.sb("rs", [128, 8], F32)
            tmps = ln_tmps()
            YAc = [[TB(f"yac{s_}_{c}") for c in range(16)] for s_ in range(3)]
            HTc = [[TB(f"htc{s_}_{c}") for c in range(16)] for s_ in range(3)]
            dcnt = [0]

            def block_subs(b):
                subs_ = [(b * 1024, 512, 0), (b * 1024 + 512, 512, 512)]
                if b == nblk - 1:
                    subs_.append((NR, NM, 1024))
                return subs_

            def load_sub(b, si):
                (t0, n, c0) = block_subs(b)[si]
                k.dma(SP, hT[:, :, c0:c0 + n], fm(HTb[:, t0:t0 + n]), [], HTc[si], HT_.d)
                k.dma(SP, ya[:, :, c0:c0 + n], fm(HT32[:, t0:t0 + n]), [], YAc[si], YA.d)
            prefetched = set()
            if last:
                osts = [k.sb(f"ost{i}", [128, 1024], F32, dma=True) for i in range(1)]
            nblk = NR // 1024
            wcnt = [0, 0]
            for b in range(nblk):
                subs = block_subs(b)
                for si in range(len(subs)):
                    if (b, si) not in prefetched:
                        load_sub(b, si)
                for si, (t0, n, c0) in enumerate(subs):
                    for j in range((n + 127) // 128):
                        p = min(128, n - j * 128)
                        cc = c0 + j * 128
                        mm(PT[0], ps[0][0:p, 0:NE], [(ya[:, kc, cc:cc + p], wrt[:, kc, :]) for kc in range(16)], list(YAc[si]))
                        S_, SEL_, M1, EQ, S2, M2, MSK, IS1, IS2 = [rt[0:p, i, :] for i in range(9)]
                        k.op(ACT, [PT[0]], [RT], lambda e: e.activation(out=S_, in_=ps[0][0:p, 0:NE], func=AF.Sigmoid))
                        k.op(DVE, [RT] + CR, [RT], lambda e: e.tensor_tensor(out=SEL_, in0=S_, in1=rb_bc[0:p, :], op=ALU.add))
                        sel3 = SEL_.rearrange("p (g e) -> p g e", e=4)
                        k.op(DVE, [RT], [RS], lambda e: e.tensor_reduce(out=rs[0:p, 0:4], in_=sel3, axis=AX.X, op=ALU.max))
                        k.op(DVE, [RT, RS], [RT], lambda e: e.tensor_tensor(out=EQ.rearrange("p (g e) -> p g e", e=4), in0=sel3, in1=rs[0:p, 0:4].unsqueeze(2).to_broadcast([p, 4, 4]), op=ALU.is_equal))
                        k.op(DVE, [RT], [RT], lambda e: e.scalar_tensor_tensor(out=S2, in0=EQ, scalar=NEG, in1=SEL_, op0=ALU.mult, op1=ALU.add))
                        k.op(DVE, [RT], [RS], lambda e: e.tensor_reduce(out=rs[0:p, 4:8], in_=S2.rearrange("p (g e) -> p g e", e=4), axis=AX.X, op=ALU.max))
                        k.op(DVE, [RS], [RS], lambda e: e.tensor_tensor(out=rs[0:p, 0:4], in0=rs[0:p, 0:4], in1=rs[0:p, 4:8], op=ALU.add))
                        k.op(DVE, [RS], [RS], lambda e: e.tensor_reduce(out=rs[0:p, 4:5], in_=rs[0:p, 0:4], axis=AX.X, op=ALU.max))
                        k.op(DVE, [RS], [RS], lambda e: e.tensor_scalar(out=rs[0:p, 0:4], in0=rs[0:p, 0:4], scalar1=rs[0:p, 4:5], scalar2=None, op0=ALU.is_equal))
                        k.op(DVE, [RS], [RS], lambda e: e.tensor_scalar(out=rs[0:p, 0:4], in0=rs[0:p, 0:4], scalar1=-1.0, scalar2=-NEG, op0=ALU.add, op1=ALU.mult))
                        k.op(DVE, [RT, RS], [RT], lambda e: e.tensor_tensor(out=MSK.rearrange("p (g e) -> p g e", e=4), in0=sel3, in1=rs[0:p, 0:4].unsqueeze(2).to_broadcast([p, 4, 4]), op=ALU.add))
                        k.op(DVE, [RT], [RS], lambda e: e.tensor_reduce(out=rs[0:p, 5:6], in_=MSK, axis=AX.X, op=ALU.max))
                        k.op(DVE, [RT, RS], [RT], lambda e: e.tensor_scalar(out=IS1, in0=MSK, scalar1=rs[0:p, 5:6], scalar2=None, op0=ALU.is_equal))
                        k.op(DVE, [RT], [RT], lambda e: e.scalar_tensor_tensor(out=MSK, in0=IS1, scalar=NEG, in1=MSK, op0=ALU.mult, op1=ALU.add))
                        k.op(DVE, [RT], [RS], lambda e: e.tensor_reduce(out=rs[0:p, 6:7], in_=MSK, axis=AX.X, op=ALU.max))
                        k.op(DVE, [RT, RS], [RT], lambda e: e.tensor_scalar(out=IS2, in0=MSK, scalar1=rs[0:p, 6:7], scalar2=None, op0=ALU.is_equal))
                        k.op(DVE, [RT], [RT], lambda e: e.tensor_tensor(out=IS1, in0=IS1, in1=IS2, op=ALU.add))
                        k.op(DVE, [RT], [RT], lambda e: e.tensor_tensor(out=IS1, in0=IS1, in1=S_, op=ALU.mult))
                        k.op(DVE, [RT], [RS], lambda e: e.tensor_reduce(out=rs[0:p, 7:8], in_=IS1, axis=AX.X, op=ALU.add))
                        k.op(DVE, [RS], [RS], lambda e: e.reciprocal(out=rs[0:p, 7:8], in_=rs[0:p, 7:8]))
                        k.op(DVE, [RT, RS], [RT], lambda e: e.tensor_scalar(out=IS2, in0=IS1, scalar1=rs[0:p, 7:8], scalar2=1.0 / ALPHA, op0=ALU.mult, op1=ALU.mult))
                        k.op(PE, [RT] + CR, [PT[1]], lambda e: e.transpose(ps[1][0:NE, 0:p], IS2, ident[0:p, 0:p]))
                        k.op(ACT, [PT[1]], [CMBT], lambda e: e.activation(out=cmbT[:, cc:cc + p], in_=ps[1][0:NE, 0:p], func=AF.Identity))
                def load_gu(i):
                    if i >= NE * 4:
                        return
                    ex_, q_ = divmod(i, 4)
                    g_, G_ = wg[i % 2]
                    u_, U_ = wu[i % 2]
                    k.dma(POOL, g_[:], fm(w_gate[l, ex_, :, q_ * 256:(q_ + 1) * 256]), [], [G_], G_.d)
                    k.dma(POOL, u_[:], fm(w_up[l, ex_, :, q_ * 256:(q_ + 1) * 256]), [], [U_], U_.d)

                def load_d(i):
                    if i >= NE * 4:
                        return
                    ex_, q_ = divmod(i, 4)
                    d_, D_ = wd[i % 2]
                    k.dma(POOL, d_[:], w_down[l, ex_, :, q_ * 512:(q_ + 1) * 512].rearrange("(fc p) f -> p fc f", p=128), [], [D_], D_.d)
                for ex in range(NE):
                    cb, CB = cbs[ex % 2]
                    for si, (t0, n, c0) in enumerate(subs):
                        u = 6 + si % 2
                        mm(PT[u], ps[u][:, 0:n], [(sel[:, ex * 128:(ex + 1) * 128], cmbT[:, c0:c0 + n])], [CMBT])
                        k.op(ACT, [PT[u]], [CB], lambda e, u=u, c0=c0, n=n, cb=cb: e.activation(out=cb[:, c0:c0 + n], in_=ps[u][:, 0:n], func=AF.Identity))
                    for q4 in range(4):
                        if ex == 0 and q4 == 0:
                            load_gu(0)
                        load_gu(ex * 4 + q4 + 1)
                        g_, G_ = wg[(ex * 4 + q4) % 2]
                        u_, U_ = wu[(ex * 4 + q4) % 2]
                        for f2 in range(2):
                            ffc = q4 * 2 + f2
                            for si, (t0, n, c0) in enumerate(subs):
                                pg, pu = si % 2, 2 + si % 2
                                mm(PT[pg], ps[pg][:, 0:n], [(g_[:, kc, f2 * 128:(f2 + 1) * 128], hT[:, kc, c0:c0 + n]) for kc in range(16)], [G_] + HTc[si])
                                mm(PT[pu], ps[pu][:, 0:n], [(u_[:, kc, f2 * 128:(f2 + 1) * 128], hT[:, kc, c0:c0 + n]) for kc in range(16)], [U_] + HTc[si])
                                sg, SG = sgs[(ffc * 3 + si) % 3]
                                tt, TT = tts[(ffc * 3 + si) % 3]
                                k.op(ACT, [PT[pg]], [SG], lambda e, pg=pg, sg=sg, n=n: e.activation(out=sg[:, 0:n], in_=ps[pg][:, 0:n], func=AF.Silu))
                                k.op(DVE, [PT[pu], CB], [TT], lambda e, pu=pu, tt=tt, n=n, c0=c0, cb=cb: e.tensor_tensor(out=tt[:, 0:n], in0=ps[pu][:, 0:n], in1=cb[:, c0:c0 + n], op=ALU.mult))
                                k.op(DVE, [SG, TT], [HE], lambda e, sg=sg, tt=tt, n=n, c0=c0, ffc=ffc: e.tensor_tensor(out=he[:, ffc, c0:c0 + n], in0=sg[:, 0:n], in1=tt[:, 0:n], op=ALU.mult))
                    for q4 in range(4):
                        if ex == 0 and q4 == 0:
                            load_d(0)
                        load_d(ex * 4 + q4 + 1)
                        d_, D_ = wd[(ex * 4 + q4) % 2]
                        for f4 in range(4):
                            fc = q4 * 4 + f4
                            for si, (t0, n, c0) in enumerate(subs):
                                pd = 4 + dcnt[0] % 4
                                dcnt[0] += 1
                                mm(PT[pd], ps[pd][:, 0:n], [(d_[:, ffc, f4 * 128:(f4 + 1) * 128], he[:, ffc, c0:c0 + n]) for ffc in range(8)], [D_, HE])
                                k.op(DVE, [PT[pd], YAc[si][fc]], [YAc[si][fc]], lambda e, pd=pd, fc=fc, n=n, c0=c0: e.tensor_tensor(out=ya[:, fc, c0:c0 + n], in0=ya[:, fc, c0:c0 + n], in1=ps[pd][:, 0:n], op=ALU.add))
                for si, (t0, n, c0) in enumerate(subs):
                    ln_fm(n, lambda kc, c0=c0, n=n: ya[:, kc, c0:c0 + n], YAc[si],
                          (None if last else (lambda kc, c0=c0, n=n: hT[:, kc, c0:c0 + n])), HTc[si],
                          co + C_L2G, co + C_L2B, eps2c[:, :], tmps)
                    if not last:
                        k.dma(SP, fm(HT32[:, t0:t0 + n]), ya[:, :, c0:c0 + n], YAc[si], [], YA.d)
                        k.dma(SP, fm(HTb[:, t0:t0 + n]), hT[:, :, c0:c0 + n], HTc[si], [], HT_.d)
                        if b + 1 < nblk and si < 2:
                            load_sub(b + 1, si)
                            prefetched.add((b + 1, si))
                    elif t0 < NR:
                        for j in range(n // 128):
                            os_, OS_ = osts[0]
                            s, s0 = divmod(t0 + j * 128, SEQ)
                            for b4 in range(4):
                                def tr(e, b4=b4, j=j, c0=c0):
                                    ins = None
                                    for q in range(4):
                                        kc = b4 * 4 + q
                                        ins = e.transpose(ps[b4][:, q * 128:(q + 1) * 128], ya[:, kc, c0 + j * 128:c0 + (j + 1) * 128], ident[:, :])
                                    return ins
                                k.op(PE, YAc[si] + CR, [PT[b4]], tr)
                                hb4 = b4 % 2
                                if b4 % 2 == 0:
                                    k.op(ACT, [PT[b4]], [OS_], lambda e, b4=b4, os_=os_, hb4=hb4: e.activation(out=os_[:, hb4 * 512:(hb4 + 1) * 512], in_=ps[b4][:, :], func=AF.Identity))
                                else:
                                    k.op(DVE, [PT[b4]], [OS_], lambda e, b4=b4, os_=os_, hb4=hb4: e.tensor_copy(out=os_[:, hb4 * 512:(hb4 + 1) * 512], in_=ps[b4][:, :]))
                                    hf = b4 // 2
                                    k.dma(SP, out[s, s0:s0 + 128, hf * 1024:(hf + 1) * 1024], os_[:, :], [OS_], [], OS_.d)
            k.end()

        stop = dbg if isinstance(dbg, str) else None
        if stop == "const":
            k.barrier()
            return nc
        phase0()
        for l in range(depth):
            if stop == "p0":
                break
            phaseA1(l)
            phaseA2(l)
            if stop == "a":
                break
            phaseB1(l)
            if stop == "b1":
                break
            phaseB2(l)
            if stop == "b2":
                break
            phaseC(l, l == depth - 1)
    return nc


def _rope_tables(SPC):
    NR, NM = SPC * SEQ, SPC * NMETA
    ROWS = SEQ // 64
    pr = np.concatenate([np.tile(np.repeat(np.arange(ROWS, dtype=np.float32), 64), SPC), np.full((NM,), -1.0, np.float32)])
    pc = np.concatenate([np.tile(np.tile(np.arange(64, dtype=np.float32), ROWS), SPC), np.tile(np.arange(NMETA, dtype=np.float32), SPC)])

    def tab(rot):
        n = rot // 4
        inv = (np.float32(10000.0) ** (-np.arange(n, dtype=np.float32) / np.float32(n))).astype(np.float32)
        ang = np.concatenate([pr[:, None] * inv, pc[:, None] * inv], axis=-1).astype(np.float32)
        c, s = np.cos(ang).T.astype(np.float32), np.sin(ang).T.astype(np.float32)
        reps = 128 // (rot // 2)
        return np.ascontiguousarray(np.stack([np.concatenate([c] * reps, 0), np.concatenate([s] * reps, 0)], 0))
    return tab(64), tab(128)


def _rmats():
    def R(half):
        m = np.zeros((2 * half, 2 * half), np.float32)
        for i in range(half):
            m[i + half, i] = -1.0
            m[i, i + half] = 1.0
        return m
    ra = np.zeros((128, 128), np.float32)
    ra[0:64, 0:64] = R(32)
    ra[64:128, 64:128] = R(32)
    return np.stack([ra, R(64)], 0)


def _prep_shared(inp, depth):
    f = lambda a: np.ascontiguousarray(np.asarray(a, dtype=np.float32))
    qperm = np.concatenate([np.arange(h * 192, h * 192 + 128) for h in range(8)] + [np.arange(h * 192 + 128, h * 192 + 192) for h in range(8)])
    kperm = np.concatenate([np.arange(h * 256, h * 256 + 128) for h in range(8)] + [np.arange(h * 256 + 128, h * 256 + 256) for h in range(8)])
    cols = np.zeros((128, depth * NCOL), np.float32)

    def put(l, off, v):
        v = np.asarray(v, np.float32)
        c = v.shape[0] // 128
        cols[:, l * NCOL + off:l * NCOL + off + c] = v.reshape(c, 128).T
    for l in range(depth):
        put(l, C_GQ, inp["g_q_lora"][l]); put(l, C_GKV, inp["g_kv_lora"][l])
        put(l, C_GQQ, inp["g_qk_q"][l]); put(l, C_GQK, inp["g_qk_k"][l])
        put(l, C_GOA, inp["g_out_mla"][l]); put(l, C_GOB, inp["g_out_gqa"][l])
        put(l, C_L1G, inp["ln1_g"][l]); put(l, C_L1B, inp["ln1_b"][l])
        put(l, C_L2G, inp["ln2_g"][l]); put(l, C_L2B, inp["ln2_b"][l])
    sel = np.zeros((NE, NE, 128), np.float32)
    for e in range(NE):
        sel[e, e, :] = 1.0
    return {
        "meta": f(inp["meta_tokens"]), "lng": f(inp["ln_in_g"]), "lnb": f(inp["ln_in_b"]),
        "w_in": f(inp["w_in"][:depth]), "w_qb": f(np.asarray(inp["w_q_b"])[:depth][:, :, qperm]),
        "w_kvb": f(np.asarray(inp["w_kv_b"])[:depth][:, :, kperm]), "w_out": f(inp["w_out"][:depth]),
        "w_rt": f(inp["w_router"]), "rbias": f(inp["router_bias"]),
        "w_gate": f(inp["w_gate"][:depth]), "w_up": f(inp["w_up"][:depth]), "w_down": f(inp["w_down"][:depth]),
        "cols": cols, "ident": np.eye(128, dtype=np.float32), "rmat": _rmats(), "sel": sel.reshape(NE, NE * 128),
    }


def kernel(**inputs):
    SPC = SPC_DEFAULT
    ncores = 16 // SPC
    x = np.asarray(inputs["x"], dtype=np.float32)
    shared = _prep_shared(inputs, DEPTH)
    tA, tB = _rope_tables(SPC)
    shared["tabA"], shared["tabB"] = tA, tB
    nc = build_nc(SPC, DEPTH)
    in_maps = []
    for c in range(ncores):
        m = dict(shared)
        m["x"] = np.ascontiguousarray(x[c * SPC:(c + 1) * SPC])
        in_maps.append(m)
    res = run_bass_kernel_spmd(nc, in_maps, core_ids=list(range(ncores)))
    return np.concatenate([r["out"] for r in res.results], axis=0).astype(np.float32)
```
